# Optimizing a Trainium2 kernel written in Bass

```python
import math
import jax, jax.numpy as jnp
from jax import lax
import numpy as np

D_MODEL = 1024
BATCH = 8
SEQ = 8192
DEPTH = 1

MEM_LEN = 256
SSD_HEADS = 16
SSD_HEAD_DIM = 64
SSD_INNER = SSD_HEADS * SSD_HEAD_DIM
SSD_GROUPS = 2
SSD_HEADS_PER_GROUP = SSD_HEADS // SSD_GROUPS
SSD_STATE = 128
SSD_CONV = 4
SSD_CONV_DIM = SSD_INNER + 2 * SSD_GROUPS * SSD_STATE
SSD_CHUNK = 128
RET_HEADS = 8
RET_QK_DIM = 64
RET_V_DIM = 128
RET_QK = RET_HEADS * RET_QK_DIM
RET_INNER = RET_HEADS * RET_V_DIM
RET_CHUNK = 128
ROPE_BASE = 10000.0
MIX_WIDTH = SSD_INNER + RET_INNER
IN_SIZES = (SSD_INNER, SSD_CONV_DIM, SSD_HEADS, RET_QK, RET_QK, RET_INNER, RET_INNER)
IN_COLS = SSD_INNER + SSD_CONV_DIM + SSD_HEADS + 2 * RET_QK + 2 * RET_INNER
XATTN_HEADS = 4
XATTN_HEAD_DIM = D_MODEL // XATTN_HEADS
D_FF = 2752
FFN_CONV = 3
ALPHA = (2.0 * DEPTH) ** 0.25
BETA = (8.0 * DEPTH) ** -0.25
EPS = 1e-5

kernel_name = "hybrid_ssd_retention_deepnorm_layer"


def _layer_norm(x, g, b):
    xf = x.astype(jnp.float32)
    mu = jnp.mean(xf, axis=-1, keepdims=True)
    var = jnp.mean(jnp.square(xf - mu), axis=-1, keepdims=True)
    return ((xf - mu) * lax.rsqrt(var + EPS) * g.astype(jnp.float32) + b.astype(jnp.float32)).astype(x.dtype)


def _split_cols(u, sizes):
    offs, acc = [], 0
    for s in sizes[:-1]:
        acc += s
        offs.append(acc)
    return jnp.split(u, offs, axis=-1)


def _causal_dwconv(u, w, b):
    k, ch = w.shape
    y = lax.conv_general_dilated(
        u, w[:, None, :].astype(u.dtype), window_strides=(1,), padding=[(k - 1, 0)],
        dimension_numbers=("NWC", "WIO", "NWC"), feature_group_count=ch)
    return y + b.astype(u.dtype)


def _ssd_chunked(xh, dt, a_head, b_in, c_in):
    bsz, seqlen, ng, nr, hp = xh.shape
    ns = b_in.shape[-1]
    q = SSD_CHUNK
    nc = seqlen // q
    f32 = jnp.float32
    xdt = (xh.astype(f32) * dt[..., None]).reshape(bsz, nc, q, ng, nr, hp)
    a = jnp.moveaxis((dt * a_head).reshape(bsz, nc, q, ng, nr), 2, -1)
    a_cs = jnp.cumsum(a, axis=-1)
    bc = b_in.astype(f32).reshape(bsz, nc, q, ng, ns)
    cc = c_in.astype(f32).reshape(bsz, nc, q, ng, ns)
    causal = jnp.tril(jnp.ones((q, q), dtype=bool))
    seg = jnp.exp(jnp.where(causal, a_cs[..., :, None] - a_cs[..., None, :], -jnp.inf))
    cb = jnp.einsum("bclgn,bcsgn->bcgls", cc, bc)
    att = cb[:, :, :, None] * seg
    y_diag = jnp.einsum("bcgrls,bcsgrp->bclgrp", att, xdt)
    decay_to_end = jnp.moveaxis(jnp.exp(a_cs[..., -1:] - a_cs), -1, 2)
    states = jnp.einsum("bcsgn,bcsgrp->bcgrpn", bc, xdt * decay_to_end[..., None])
    chunk_decay = jnp.exp(a_cs[..., -1])

    def step(h, inp):
        st, dec = inp
        return h * dec[..., None, None] + st, h

    h0 = jnp.zeros((bsz, ng, nr, hp, ns), f32)
    _, h_prev = lax.scan(step, h0, (jnp.moveaxis(states, 1, 0), jnp.moveaxis(chunk_decay, 1, 0)))
    h_prev = jnp.moveaxis(h_prev, 0, 1)
    decay_from_start = jnp.moveaxis(jnp.exp(a_cs), -1, 2)
    y_off = jnp.einsum("bclgn,bcgrpn->bclgrp", cc, h_prev) * decay_from_start[..., None]
    return (y_diag + y_off).reshape(bsz, seqlen, ng, nr, hp)


def _rotary(u, cos, sin):
    u = u.astype(jnp.float32)
    u1, u2 = jnp.split(u, 2, axis=-1)
    c = cos[None, :, None, :]
    s = sin[None, :, None, :]
    return jnp.concatenate([u1 * c - u2 * s, u1 * s + u2 * c], axis=-1)


def _retention_chunkwise(q, k, v, log_gamma):
    bsz, seqlen, nh, dk = q.shape
    dv = v.shape[-1]
    n = RET_CHUNK
    nc = seqlen // n
    f32 = jnp.float32
    qc = q.reshape(bsz, nc, n, nh, dk)
    kc = k.reshape(bsz, nc, n, nh, dk)
    vc = v.astype(f32).reshape(bsz, nc, n, nh, dv)
    pos = jnp.arange(n, dtype=f32)
    dist = pos[:, None] - pos[None, :]
    decay = jnp.exp(jnp.where(dist >= 0, dist * log_gamma[:, None, None], -jnp.inf))
    s = jnp.einsum("bcnhd,bcmhd->bchnm", qc, kc) * decay
    y_in = jnp.einsum("bchnm,bcmhe->bcnhe", s, vc)
    k_dec = jnp.exp((n - 1 - pos)[:, None] * log_gamma)
    states = jnp.einsum("bcmhd,bcmhe->bchde", kc * k_dec[:, :, None], vc)
    chunk_decay = jnp.exp(n * log_gamma)

    def step(r, st):
        return r * chunk_decay[:, None, None] + st, r

    r0 = jnp.zeros((bsz, nh, dk, dv), f32)
    _, r_prev = lax.scan(step, r0, jnp.moveaxis(states, 1, 0))
    r_prev = jnp.moveaxis(r_prev, 0, 1)
    q_dec = jnp.exp((pos + 1.0)[:, None] * log_gamma)
    y_cross = jnp.einsum("bcnhd,bchde->bcnhe", qc * q_dec[:, :, None], r_prev)
    return (y_in + y_cross).reshape(bsz, seqlen, nh, dv)


def _mixer(x, w_in, conv_w, conv_b, dt_bias, a_log, d_skip, norm_w, gn_w, gn_b, w_out, cos, sin):
    f32 = jnp.float32
    bsz, seqlen, _ = x.shape
    proj = x @ w_in
    z, xbc, dt_raw, q, k, v, g = _split_cols(proj, IN_SIZES)
    xbc = jax.nn.silu(_causal_dwconv(xbc, conv_w, conv_b))
    xs, b_in, c_in = _split_cols(xbc, (SSD_INNER, SSD_GROUPS * SSD_STATE, SSD_GROUPS * SSD_STATE))
    dt = jax.nn.softplus(dt_raw.astype(f32) + dt_bias.astype(f32))
    a_head = -jnp.exp(a_log.astype(f32))
    xh = xs.reshape(bsz, seqlen, SSD_GROUPS, SSD_HEADS_PER_GROUP, SSD_HEAD_DIM)
    y = _ssd_chunked(
        xh, dt.reshape(bsz, seqlen, SSD_GROUPS, SSD_HEADS_PER_GROUP),
        a_head.reshape(SSD_GROUPS, SSD_HEADS_PER_GROUP),
        b_in.reshape(bsz, seqlen, SSD_GROUPS, SSD_STATE),
        c_in.reshape(bsz, seqlen, SSD_GROUPS, SSD_STATE))
    y = y + d_skip.astype(f32).reshape(SSD_GROUPS, SSD_HEADS_PER_GROUP)[:, :, None] * xh.astype(f32)
    u = (y.reshape(bsz, seqlen, SSD_INNER) * jax.nn.silu(z.astype(f32))).reshape(bsz, seqlen, SSD_GROUPS, -1)
    u = u * lax.rsqrt(jnp.mean(jnp.square(u), axis=-1, keepdims=True) + EPS)
    y_ssd = u.reshape(bsz, seqlen, SSD_INNER) * norm_w.astype(f32)
    log_gamma = jnp.log1p(-jnp.exp2(-5.0 - jnp.arange(RET_HEADS, dtype=f32)))
    qh = _rotary(q.reshape(bsz, seqlen, RET_HEADS, RET_QK_DIM), cos, sin)
    kh = _rotary(k.reshape(bsz, seqlen, RET_HEADS, RET_QK_DIM), cos, sin) * (RET_QK_DIM ** -0.5)
    yr = _retention_chunkwise(qh, kh, v.reshape(bsz, seqlen, RET_HEADS, RET_V_DIM), log_gamma)
    mu = jnp.mean(yr, axis=-1, keepdims=True)
    var = jnp.mean(jnp.square(yr - mu), axis=-1, keepdims=True)
    yr = ((yr - mu) * lax.rsqrt(var + EPS)).reshape(bsz, seqlen, RET_INNER)
    yr = yr * gn_w.astype(f32) + gn_b.astype(f32)
    y_ret = jax.nn.silu(g.astype(f32)) * yr
    y_mix = jnp.concatenate([y_ssd, y_ret], axis=-1).astype(x.dtype)
    return y_mix @ w_out


def _memory_cross_attention(x, mem, w_q, w_k, w_v, w_o):
    bsz, seqlen, _ = x.shape
    m = mem.shape[1]
    qx = (x @ w_q).reshape(bsz, seqlen, XATTN_HEADS, XATTN_HEAD_DIM)
    km = (mem @ w_k).reshape(bsz, m, XATTN_HEADS, XATTN_HEAD_DIM)
    vm = (mem @ w_v).reshape(bsz, m, XATTN_HEADS, XATTN_HEAD_DIM)
    s = jnp.einsum("blhd,bmhd->bhlm", qx, km).astype(jnp.float32) * (XATTN_HEAD_DIM ** -0.5)
    p = jax.nn.softmax(s, axis=-1).astype(vm.dtype)
    o = jnp.einsum("bhlm,bmhd->blhd", p, vm).reshape(bsz, seqlen, D_MODEL)
    return o @ w_o


def _conv_glu_ffn(x, w_up, b_up, conv_w, conv_b, w_down):
    h = x @ w_up + b_up
    h = _causal_dwconv(h, conv_w, conv_b)
    a, u = jnp.split(h, 2, axis=-1)
    return (jax.nn.silu(a) * u) @ w_down


def setup_inputs(seed: int = 0) -> dict:
    key = jax.random.key(seed)
    ks = jax.random.split(key, 32)
    f32 = jnp.float32

    def nrm(k, shape, scale):
        return jax.random.normal(k, shape, f32) * scale

    nl = DEPTH
    dt0 = jnp.exp(jax.random.uniform(ks[5], (nl, SSD_HEADS), f32, math.log(1e-3), math.log(1e-1)))
    return {
        "x": nrm(ks[0], (BATCH, SEQ, D_MODEL), 1.0),
        "mem": nrm(ks[1], (BATCH, MEM_LEN, D_MODEL), 1.0),
        "w_in": nrm(ks[2], (nl, D_MODEL, IN_COLS), D_MODEL ** -0.5),
        "ssd_conv_w": nrm(ks[3], (nl, SSD_CONV, SSD_CONV_DIM), SSD_CONV ** -0.5),
        "ssd_conv_b": nrm(ks[4], (nl, SSD_CONV_DIM), 0.02),
        "ssd_dt_bias": dt0 + jnp.log(-jnp.expm1(-dt0)),
        "ssd_a_log": jnp.log(jax.random.uniform(ks[6], (nl, SSD_HEADS), f32, 1.0, 16.0)),
        "ssd_d": 1.0 + nrm(ks[7], (nl, SSD_HEADS), 0.02),
        "ssd_norm_w": 1.0 + nrm(ks[8], (nl, SSD_INNER), 0.02),
        "ret_gn_w": 1.0 + nrm(ks[9], (nl, RET_INNER), 0.02),
        "ret_gn_b": nrm(ks[10], (nl, RET_INNER), 0.02),
        "w_mix_out": nrm(ks[11], (nl, MIX_WIDTH, D_MODEL), BETA * MIX_WIDTH ** -0.5),
        "ln1_g": 1.0 + nrm(ks[12], (nl, D_MODEL), 0.02),
        "ln1_b": nrm(ks[13], (nl, D_MODEL), 0.02),
        "w_xq": nrm(ks[14], (nl, D_MODEL, D_MODEL), D_MODEL ** -0.5),
        "w_xk": nrm(ks[15], (nl, D_MODEL, D_MODEL), D_MODEL ** -0.5),
        "w_xv": nrm(ks[16], (nl, D_MODEL, D_MODEL), D_MODEL ** -0.5),
        "w_xo": nrm(ks[17], (nl, D_MODEL, D_MODEL), BETA * D_MODEL ** -0.5),
        "ln2_g": 1.0 + nrm(ks[18], (nl, D_MODEL), 0.02),
        "ln2_b": nrm(ks[19], (nl, D_MODEL), 0.02),
        "w_ffn_up": nrm(ks[20], (nl, D_MODEL, 2 * D_FF), D_MODEL ** -0.5),
        "b_ffn_up": nrm(ks[21], (nl, 2 * D_FF), 0.02),
        "ffn_conv_w": nrm(ks[22], (nl, FFN_CONV, 2 * D_FF), FFN_CONV ** -0.5),
        "ffn_conv_b": nrm(ks[23], (nl, 2 * D_FF), 0.02),
        "w_ffn_down": nrm(ks[24], (nl, D_FF, D_MODEL), BETA * D_FF ** -0.5),
        "ln3_g": 1.0 + nrm(ks[25], (nl, D_MODEL), 0.02),
        "ln3_b": nrm(ks[26], (nl, D_MODEL), 0.02),
    }


def reference(x, mem, w_in, ssd_conv_w, ssd_conv_b, ssd_dt_bias, ssd_a_log, ssd_d, ssd_norm_w,
              ret_gn_w, ret_gn_b, w_mix_out, ln1_g, ln1_b, w_xq, w_xk, w_xv, w_xo, ln2_g, ln2_b,
              w_ffn_up, b_ffn_up, ffn_conv_w, ffn_conv_b, w_ffn_down, ln3_g, ln3_b):
    seqlen = x.shape[1]
    pos = jnp.arange(seqlen, dtype=jnp.float32)
    freqs = 1.0 / (ROPE_BASE ** jnp.linspace(0.0, 1.0, RET_QK_DIM // 2, dtype=jnp.float32))
    ang = pos[:, None] * freqs[None, :]
    cos, sin = jnp.cos(ang), jnp.sin(ang)
    for l in range(DEPTH):
        mix = _mixer(x, w_in[l], ssd_conv_w[l], ssd_conv_b[l], ssd_dt_bias[l], ssd_a_log[l], ssd_d[l],
                     ssd_norm_w[l], ret_gn_w[l], ret_gn_b[l], w_mix_out[l], cos, sin)
        x = _layer_norm(ALPHA * x + mix, ln1_g[l], ln1_b[l])
        xa = _memory_cross_attention(x, mem, w_xq[l], w_xk[l], w_xv[l], w_xo[l])
        x = _layer_norm(ALPHA * x + xa, ln2_g[l], ln2_b[l])
        ff = _conv_glu_ffn(x, w_ffn_up[l], b_ffn_up[l], ffn_conv_w[l], ffn_conv_b[l], w_ffn_down[l])
        x = _layer_norm(ALPHA * x + ff, ln3_g[l], ln3_b[l])
    return x
```

```python
import numpy as np
from contextlib import ExitStack
import concourse.bass as bass
import concourse.mybir as mybir
from concourse.bass_utils import run_bass_kernel_spmd

F32 = mybir.dt.float32
BF16 = mybir.dt.bfloat16
AF = mybir.ActivationFunctionType
ALU = mybir.AluOpType
AX = mybir.AxisListType

D = 1024
L = 8192
T = 512
NTILES = L // T
MEM = 256
DFF = 2752
ALPHA = 2.0 ** 0.25
EPS = 1e-5
OFF_Z, OFF_XBC, OFF_DT, OFF_Q, OFF_K, OFF_V, OFF_G = 0, 1024, 2560, 2576, 3088, 3600, 4624
NSLOT = 5
SLOT = 2048
EPOCH = 16000

_CST = {}
_off = 0
for _n, _w in [("ident", 128), ("tri", 128), ("strict", 128), ("ones", 128), ("prot", 128),
               ("decT", 1024), ("qdecT", 512), ("kdec", 8), ("cdr", 4),
               ("ln1g", 1024), ("ln1b", 1024), ("ln2g", 1024), ("ln2b", 1024), ("ln3g", 1024), ("ln3b", 1024),
               ("dtb", 16), ("alog", 16), ("drow", 16), ("scb", 12), ("normw", 8), ("gnw", 8), ("gnb", 8),
               ("bup", 44), ("fcb", 44)]:
    _CST[_n] = (_off, _w)
    _off += _w
NCST = _off


def _stream_units():
    u = []
    for j in range(12):
        u.append(("xbc", j, 1024 + 512))
    for j in range(8):
        u.append(("qk", j, 1024))
    for j in range(8):
        u.append(("g", j, 1024))
    for blk in range(2):
        for hk in range(2):
            u.append(("z", (blk, hk), 2048))
    for blk in range(2):
        for hk in range(2):
            u.append(("v", (blk, hk), 2048))
    u.append(("dt", 0, 128))
    for half in range(2):
        for q in range(4):
            u.append(("wout", (half, q), 2048))
    for j in range(8):
        u.append(("xq", j, 1024))
    for half in range(2):
        for q in range(2):
            u.append(("xo", (half, q), 2048))
    for j in range(22):
        u.append(("fa", j, 1024 + 384))
        u.append(("fu", j, 1024 + 384))
    for half in range(2):
        for q in range(6):
            u.append(("down", (half, q), 2048))
    return u


UNITS = _stream_units()
UOFF = np.cumsum([0] + [x[2] for x in UNITS]).tolist()
WLEN = UOFF[-1]
ONCE_UNITS = [("xk", j, 1024) for j in range(8)] + [("xv", (b, h), 2048) for b in range(2) for h in range(2)]
OOFF = np.cumsum([0] + [x[2] for x in ONCE_UNITS]).tolist()
WOLEN = OOFF[-1]


def _fm(w, col0, ncols=128):
    K = w.shape[0]
    blk = np.zeros((K, 128), np.float32)
    c1 = min(col0 + ncols, w.shape[1])
    blk[:, : c1 - col0] = w[:, col0:c1]
    return blk.reshape(K // 128, 128, 128).transpose(1, 0, 2).reshape(128, -1)


def _tm(w, kc0, nk, col0):
    out = np.zeros((128, nk, 512), np.float32)
    for kc in range(nk):
        r0 = (kc0 + kc) * 128
        r1 = min(r0 + 128, w.shape[0])
        if r1 > r0:
            out[: r1 - r0, kc, :] = w[r0:r1, col0:col0 + 512]
    return out.reshape(128, -1)


def _diag(cw, col0):
    nt = cw.shape[0]
    out = np.zeros((128, nt, 128), np.float32)
    n = min(128, cw.shape[1] - col0)
    idx = np.arange(n)
    for k in range(nt):
        out[idx, k, idx] = cw[k, col0:col0 + n]
    return out.reshape(128, -1)


def _pp(v, n):
    buf = np.zeros(n * 128, np.float32)
    buf[: v.shape[0]] = v
    return buf.reshape(n, 128).T


def _host_prep(inp):
    f = lambda k: np.asarray(inp[k], np.float32)[0]
    w_in, w_out = f("w_in"), f("w_mix_out")
    ws = np.zeros((128, WLEN), np.float32)
    for i, (kind, key, n) in enumerate(UNITS):
        o = UOFF[i]
        if kind == "xbc":
            ws[:, o:o + 1024] = _fm(w_in, OFF_XBC + key * 128)
            ws[:, o + 1024:o + 1536] = _diag(f("ssd_conv_w"), key * 128)
        elif kind == "qk":
            ws[:, o:o + n] = _fm(w_in, OFF_Q + key * 128)
        elif kind == "g":
            ws[:, o:o + n] = _fm(w_in, OFF_G + key * 128)
        elif kind == "z":
            ws[:, o:o + n] = _tm(w_in, key[1] * 4, 4, OFF_Z + key[0] * 512)
        elif kind == "v":
            ws[:, o:o + n] = _tm(w_in, key[1] * 4, 4, OFF_V + key[0] * 512)
        elif kind == "dt":
            ws[:, o:o + n] = w_in[:, OFF_DT:OFF_DT + 16].reshape(8, 128, 16).transpose(1, 0, 2).reshape(128, -1)
        elif kind == "wout":
            ws[:, o:o + n] = _tm(w_out, key[1] * 4, 4, key[0] * 512)
        elif kind == "xq":
            ws[:, o:o + n] = _fm(f("w_xq"), key * 128)
        elif kind == "xo":
            ws[:, o:o + n] = _tm(f("w_xo"), key[1] * 4, 4, key[0] * 512)
        elif kind == "fa":
            ws[:, o:o + 1024] = _fm(f("w_ffn_up")[:, :DFF], key * 128)
            ws[:, o + 1024:o + 1408] = _diag(f("ffn_conv_w")[:, :DFF], key * 128)
        elif kind == "fu":
            ws[:, o:o + 1024] = _fm(f("w_ffn_up")[:, DFF:], key * 128)
            ws[:, o + 1024:o + 1408] = _diag(f("ffn_conv_w")[:, DFF:], key * 128)
        elif kind == "down":
            ws[:, o:o + n] = _tm(f("w_ffn_down"), key[1] * 4, 4, key[0] * 512)
    wo = np.zeros((128, WOLEN), np.float32)
    for i, (kind, key, n) in enumerate(ONCE_UNITS):
        o = OOFF[i]
        if kind == "xk":
            wo[:, o:o + n] = _fm(f("w_xk"), key * 128)
        else:
            wo[:, o:o + n] = _tm(f("w_xv"), key[1] * 4, 4, key[0] * 512)

    cst = np.zeros((128, NCST), np.float32)

    def put(name, arr):
        o, w = _CST[name]
        cst[:, o:o + w] = np.asarray(arr, np.float32).reshape(128, w)

    ar = np.arange(128)
    put("ident", np.eye(128))
    put("tri", (ar[:, None] <= ar[None, :]))
    put("strict", (ar[:, None] > ar[None, :]))
    put("ones", np.ones((128, 128)))
    prot = np.zeros((128, 128))
    for m in range(128):
        if (m % 64) < 32:
            prot[m + 32, m] = -1.0
        else:
            prot[m - 32, m] = 1.0
    put("prot", prot)
    gam = 1.0 - np.exp2(-5.0 - np.arange(8, dtype=np.float64))
    decT = np.zeros((128, 8, 128))
    for h in range(8):
        dist = ar[None, :] - ar[:, None]
        decT[:, h, :] = np.where(dist >= 0, 0.125 * gam[h] ** np.maximum(dist, 0), 0.0)
    put("decT", decT)
    qd = np.zeros((128, 4, 128))
    cdr = np.zeros((128, 4))
    for p in range(128):
        for j in range(4):
            h = 2 * j + p // 64
            qd[p, j, :] = gam[h] ** (ar + 1.0)
            cdr[p, j] = gam[h] ** 128.0
    put("qdecT", qd)
    put("cdr", cdr)
    put("kdec", 0.125 * gam[None, :] ** (127.0 - ar[:, None]))
    for nm, key in (("ln1g", "ln1_g"), ("ln1b", "ln1_b"), ("ln2g", "ln2_g"), ("ln2b", "ln2_b"),
                    ("ln3g", "ln3_g"), ("ln3b", "ln3_b")):
        put(nm, np.broadcast_to(f(key)[None, :], (128, 1024)))
    put("dtb", np.broadcast_to(f("ssd_dt_bias")[None, :], (128, 16)))
    put("alog", np.broadcast_to(f("ssd_a_log")[None, :], (128, 16)))
    put("drow", np.broadcast_to(f("ssd_d")[None, :], (128, 16)))
    put("scb", _pp(f("ssd_conv_b"), 12))
    put("normw", _pp(f("ssd_norm_w"), 8))
    put("gnw", _pp(f("ret_gn_w"), 8))
    put("gnb", _pp(f("ret_gn_b"), 8))
    bup, fcb = f("b_ffn_up"), f("ffn_conv_b")
    put("bup", np.concatenate([_pp(bup[:DFF], 22), _pp(bup[DFF:], 22)], axis=1))
    put("fcb", np.concatenate([_pp(fcb[:DFF], 22), _pp(fcb[DFF:], 22)], axis=1))

    pos = np.arange(L, dtype=np.float32)
    freqs = (1.0 / (np.float32(10000.0) ** np.linspace(0.0, 1.0, 32, dtype=np.float32))).astype(np.float32)
    ang = (pos[:, None] * freqs[None, :]).astype(np.float32).astype(np.float64)
    angp = ang[:, np.arange(128) % 32].T
    cs = np.zeros((NTILES, 128, 1024), np.float32)
    for t in range(NTILES):
        cs[t, :, :512] = np.cos(angp[:, t * T:(t + 1) * T])
        cs[t, :, 512:] = np.sin(angp[:, t * T:(t + 1) * T])
    return ws, wo, cst, cs


class _Reg:
    __slots__ = ("name", "w", "r", "excl")

    def __init__(self, name, excl=False):
        self.name = name
        self.w = None
        self.r = {}
        self.excl = excl


class _Eng:
    def __init__(self, kb, name, h, self_raw):
        self.kb, self.name, self.h, self.self_raw = kb, name, h, self_raw
        self.n = 0
        self.sems = []
        self.seen = {}

    def cur(self):
        e = self.n // EPOCH
        while len(self.sems) <= e:
            self.sems.append(self.kb.es.enter_context(self.kb.nc.semaphore(f"s_{self.name}_{len(self.sems)}")))
        return (self.name, e), self.sems[e], self.n % EPOCH + 1


class _KB:
    def __init__(self):
        self.nc = bass.Bass("TRN2", target_bir_lowering=False)
        self.es = ExitStack()
        nc = self.nc
        self.pe = _Eng(self, "pe", nc.tensor, False)
        self.act = _Eng(self, "act", nc.scalar, True)
        self.dve = _Eng(self, "dve", nc.vector, True)
        self.pool = _Eng(self, "pool", nc.gpsimd, True)
        self.sp = _Eng(self, "sp", nc.sync, False)
        self.dsem = {}
        self.dval = {}

    def sb(self, name, shape, dt):
        return self.es.enter_context(self.nc.sbuf_tensor("sb_" + name, shape, dt))

    def _waits(self, E, R, W):
        deps = {}

        def add(tok, kind):
            if tok is None:
                return
            key, sem, val, src = tok
            if src is E and (kind == "rr" or not E.self_raw):
                return
            if key not in deps or deps[key][1] < val:
                deps[key] = (sem, val)

        for r in R:
            add(r.w, "raw")
            if r.excl:
                for t in r.r.values():
                    add(t, "rr")
        for w in W:
            add(w.w, "waw")
            for t in w.r.values():
                add(t, "war")
        for key, (sem, val) in deps.items():
            if E.seen.get(key, 0) < val:
                E.h.wait_ge(sem, val)
                E.seen[key] = val

    def _mark(self, tok, R, W):
        key = tok[0]
        for r in R:
            old = r.r.get(key)
            if old is None or old[2] < tok[2]:
                r.r[key] = tok
        for w in W:
            w.w = tok
            w.r = {}

    def op(self, E, fn, R=(), W=()):
        self._waits(E, R, W)
        inst = fn()
        key, sem, val = E.cur()
        inst.then_inc(sem, 1)
        E.n += 1
        self._mark((key, sem, val, E), R, W)

    def dma(self, Q, semname, out, in_, R=(), W=()):
        self._waits(Q, R, W)
        if semname not in self.dsem:
            self.dsem[semname] = self.es.enter_context(self.nc.semaphore("d_" + semname))
            self.dval[semname] = 0
        d = Q.h.dma_start(out=out, in_=in_)
        self.dval[semname] += 16
        d.then_inc(self.dsem[semname], 16)
        self._mark((("dma", semname), self.dsem[semname], self.dval[semname], None), R, W)

    def wait_all_dma(self, Q, names):
        for n in names:
            Q.h.wait_ge(self.dsem[n], self.dval[n])


class _Stop(Exception):
    pass


def _build(NT=NTILES, dbg=False, stop=None):
    kb = _KB()
    nc = kb.nc
    PE, ACT, DVE, POOL, SP = kb.pe, kb.act, kb.dve, kb.pool, kb.sp
    te, se, ve, ge = nc.tensor, nc.scalar, nc.vector, nc.gpsimd

    xT_d = nc.dram_tensor("xT", [128, 8, L], F32, kind="ExternalInput").ap()
    xn_d = nc.dram_tensor("xn", [L, D], F32, kind="ExternalInput").ap()
    memT_d = nc.dram_tensor("memT", [128, 8, MEM], F32, kind="ExternalInput").ap()
    ws_d = nc.dram_tensor("ws", [128, WLEN], F32, kind="ExternalInput").ap()
    wo_d = nc.dram_tensor("wo", [128, WOLEN], F32, kind="ExternalInput").ap()
    cst_d = nc.dram_tensor("cst", [128, NCST], F32, kind="ExternalInput").ap()
    cs_d = nc.dram_tensor("cs", [NTILES, 128, 1024], F32, kind="ExternalInput").ap()
    out_d = nc.dram_tensor("out", [L, D], F32, kind="ExternalOutput").ap()
    wsb_d = nc.dram_tensor("wsb", [128, WLEN], BF16, kind="Internal").ap()
    R_wsb = [_Reg(f"wsb{i}") for i in range(len(UNITS))]
    dbg_d = {}
    if dbg:
        dbg_d["ymix"] = nc.dram_tensor("dbg_ymix", [128, 16 * 512], BF16, kind="ExternalOutput").ap()
        dbg_d["x1"] = nc.dram_tensor("dbg_x1", [128, 4 * 1024], F32, kind="ExternalOutput").ap()
        dbg_d["x2"] = nc.dram_tensor("dbg_x2", [128, 4 * 1024], F32, kind="ExternalOutput").ap()

    sb = kb.sb
    cst = sb("cst", [128, NCST], F32)
    R_cst = _Reg("cst")

    def C(name, a=None, b=None):
        o, w = _CST[name]
        if a is None:
            return cst[:, o:o + w]
        return cst[:, o + a:o + b]

    ident_bf = sb("ident_bf", [128, 128], BF16)
    prot_bf = sb("prot_bf", [128, 128], BF16)
    arow = sb("arow", [128, 16], F32)
    R_c2 = _Reg("c2")
    ring = sb("ring", [128, NSLOT, SLOT], BF16)
    R_ring = [_Reg(f"ring{i}") for i in range(NSLOT)]
    xTb = sb("xTb", [128, 8, T], BF16)
    R_xT = [_Reg(f"xT{c}") for c in range(4)]
    P0 = sb("P0", [128, 24576], BF16)
    xbcT = P0[:, 0:6144].rearrange("p (j t) -> p j t", j=12)
    qkT = P0[:, 6144:10240].rearrange("p (j t) -> p j t", j=8)
    qsT = P0[:, 10240:12288].rearrange("p (j t) -> p j t", j=4)
    zs = P0[:, 12288:16384].rearrange("p (c n) -> p c n", c=4)
    vt = P0[:, 16384:20480].rearrange("p (c n) -> p c n", c=4)
    gT = P0[:, 20480:24576].rearrange("p (j t) -> p j t", j=8)
    R_xbc = [_Reg(f"xbc{j}") for j in range(12)]
    R_qk = [_Reg(f"qk{j}") for j in range(8)]
    R_qs = [_Reg(f"qs{j}") for j in range(4)]
    R_zs = [_Reg(f"zs{c}") for c in range(4)]
    R_v = [_Reg(f"v{c}") for c in range(4)]
    R_g = [_Reg(f"g{j}") for j in range(8)]
    qxT = P0[:, 0:4096].rearrange("p (j t) -> p j t", j=8)
    oT = P0[:, 4096:8192].rearrange("p (j t) -> p j t", j=8)
    gT2 = P0[:, 8192:8192 + 22 * 512].rearrange("p (j t) -> p j t", j=22)
    R_qx = [_Reg(f"qx{j}") for j in range(8)]
    R_oT = [_Reg(f"oT{c}") for c in range(4)]
    R_g2 = [_Reg(f"g2{j}") for j in range(22)]
    view_a = R_xbc + R_qk + R_qs + R_zs + R_v + R_g
    view_b = R_qx + R_oT + R_g2
    fence = sb("fence", [128, 2], F32)
    R_fence = _Reg("fence")

    ymixT = sb("ymixT", [128, 16, T], BF16)
    R_ym = [_Reg(f"ym{c}") for c in range(4)]
    XB = sb("XB", [128, 4, D], F32)
    R_XB = [_Reg(f"XB{c}") for c in range(4)]
    hT = sb("hT", [128, 1024], F32)
    hT_bf = sb("hT_bf", [128, 1024], BF16)
    rS = sb("rS", [128, 512], F32)
    rS_bf = sb("rS_bf", [128, 512], BF16)
    R_hT, R_hTb, R_rS, R_rSb = _Reg("hT"), _Reg("hTb"), _Reg("rS"), _Reg("rSb")
    halo_s = sb("halo_s", [128, 12, 4], BF16)
    halo_f = sb("halo_f", [128, 44, 2], BF16)
    R_hs = [_Reg(f"hs{j}") for j in range(12)]
    R_hf = [_Reg(f"hf{j}") for j in range(44)]
    kmT = sb("kmT", [128, 8, MEM], BF16)
    vm = sb("vm", [128, 2, D], BF16)
    R_km, R_vm = _Reg("km"), _Reg("vm")
    stg = sb("stg", [128, 2, 516], BF16)
    R_stg = [_Reg("stg0"), _Reg("stg1")]
    raw = sb("raw", [128, 2, 512], BF16)
    R_raw = [_Reg("raw0"), _Reg("raw1")]
    rt1 = sb("rt1", [128, 512], F32)
    rt2 = sb("rt2", [128, 512], F32)
    R_rt1, R_rt2 = _Reg("rt1"), _Reg("rt2")
    dt_all = sb("dt_all", [128, 4, 16], F32)
    dt_tmp = sb("dt_tmp", [128, 4, 16], F32)
    R_dt, R_dtt = _Reg("dt"), _Reg("dtt")
    a_sb = sb("a_sb", [128, 16], F32)
    R_a = _Reg("a")
    rhs_a = sb("rhs_a", [128, 16, 128], F32)
    R_rhsa = [_Reg("rhsa0"), _Reg("rhsa1")]
    segT = sb("segT", [128, 16, 128], BF16)
    R_seg = [_Reg("seg0"), _Reg("seg1")]
    attT = sb("attT", [128, 16, 128], BF16)
    R_att = [_Reg("att0"), _Reg("att1")]
    cbm = sb("cbm", [128, 2, 128], BF16)
    R_cbm = _Reg("cbm")
    xdt = sb("xdt", [128, 1024], BF16)
    xdtd = sb("xdtd", [128, 1024], BF16)
    R_xdt, R_xdtd = _Reg("xdt"), _Reg("xdtd")
    dtd = sb("dtd", [128, 16], F32)
    R_dtd = _Reg("dtd")
    F1 = sb("F1", [128, 1024], F32)
    F2 = sb("F2", [128, 1024], F32)
    R_F1, R_F2 = _Reg("F1"), _Reg("F2")
    cs_sb, R_cs = F2, R_F2
    H1 = sb("H1", [128, 1024], BF16)
    H2 = sb("H2", [128, 1024], BF16)
    R_H1, R_H2 = _Reg("H1"), _Reg("H2")
    Btok = sb("Btok", [128, 2, 128], BF16)
    R_Btok = _Reg("Btok")
    kd = sb("kd", [128, 512], BF16)
    R_kd = _Reg("kd")
    sT = sb("sT", [128, 8, 128], BF16)
    R_sT = _Reg("sT")
    pT = sb("pT", [128, 8, 128], BF16)
    R_pT = _Reg("pT")
    asil = sb("asil", [128, 512], BF16)
    R_asil = _Reg("asil")
    sm = sb("sm", [128, 64], F32)
    R_sm = _Reg("sm")
    R_smS, R_smR = _Reg("smS"), _Reg("smR")
    dfcd = sb("dfcd", [128, 32], F32)
    R_dfcd = _Reg("dfcd")
    bnst = sb("bnst", [128, 2, 6], F32)
    bnmv = sb("bnmv", [128, 2], F32)
    R_bn = _Reg("bn")

    ps = kb.es.enter_context(nc.psum_tensor("ps", [128, 8, 512], F32))
    R_ps = [_Reg(f"ps{i}", excl=True) for i in range(8)]

    def psb(b, a=0, e=512):
        return ps[:, b, a:e]

    def ps2(b):
        return ps[:, b:b + 2, :].rearrange("p a b -> p (a b)")

    def psbf(b):
        return ps[:, b, :].bitcast(BF16)

    state = {"next": 0, "use": 0, "once_next": 0}
    total_units = len(UNITS) * NT

    def issue_until(g):
        while state["next"] <= min(g, total_units - 1):
            n = state["next"]
            i = n % len(UNITS)
            s = n % NSLOT
            ncols = UNITS[i][2]
            if n < len(UNITS):
                kb.dma(POOL, f"ring{s}", ring[:, s, 0:ncols], ws_d[:, UOFF[i]:UOFF[i] + ncols], W=[R_ring[s]])
                if NT > 1:
                    kb.dma(SP, f"wb{s}", wsb_d[:, UOFF[i]:UOFF[i] + ncols], ring[:, s, 0:ncols],
                           R=[R_ring[s]], W=[R_wsb[i]])
            else:
                kb.dma(SP, f"ringh{s}", ring[:, s, 0:ncols], wsb_d[:, UOFF[i]:UOFF[i] + ncols],
                       R=[R_wsb[i]], W=[R_ring[s]])
            state["next"] += 1

    def unit(kind, key, hold=0):
        g = state["use"]
        i = g % len(UNITS)
        assert UNITS[i][0] == kind and UNITS[i][1] == key, (UNITS[i], kind, key)
        issue_until(g + NSLOT - 1 - hold)
        state["use"] += 1
        s = g % NSLOT
        return ring[:, s, :], R_ring[s]

    kb.dma(POOL, "cst", cst[:, :], cst_d[:, :], W=[R_cst])
    kb.op(DVE, lambda: ve.tensor_copy(out=ident_bf[:, :], in_=C("ident")), R=[R_cst], W=[R_c2])
    kb.op(DVE, lambda: ve.tensor_copy(out=prot_bf[:, :], in_=C("prot")), R=[R_cst], W=[R_c2])
    kb.op(ACT, lambda: se.activation(out=arow[:, :], in_=C("alog"), func=AF.Exp), R=[R_cst], W=[R_c2])
    kb.op(DVE, lambda: ve.tensor_scalar(out=arow[:, :], in0=arow[:, :], scalar1=-1.0, scalar2=None, op0=ALU.mult),
          R=[R_c2], W=[R_c2])
    kb.op(DVE, lambda: ve.memset(hT[:, :], 0.0), W=[R_hT])
    kb.op(DVE, lambda: ve.memset(hT_bf[:, :], 0.0), W=[R_hTb])
    kb.op(DVE, lambda: ve.memset(rS[:, :], 0.0), W=[R_rS])
    kb.op(DVE, lambda: ve.memset(rS_bf[:, :], 0.0), W=[R_rSb])
    kb.op(DVE, lambda: ve.memset(halo_s[:, :, :], 0.0), W=R_hs)
    kb.op(DVE, lambda: ve.memset(halo_f[:, :, :], 0.0), W=R_hf)
    kb.op(DVE, lambda: ve.memset(fence[:, :], 0.0), W=[R_fence])

    memTb = xTb[:, :, 0:MEM]
    kb.dma(POOL, "xT", memTb, memT_d[:, :, :], W=R_xT)
    pa_i = [0]

    def next_pa():
        b = pa_i[0] % 2
        pa_i[0] += 1
        return b

    for i, (kind, key, n) in enumerate(ONCE_UNITS):
        s = i % NSLOT
        kb.dma(POOL, f"ring{s}", ring[:, s, 0:n], wo_d[:, OOFF[i]:OOFF[i] + n], W=[R_ring[s]])
        if kind == "xk":
            b = next_pa()
            for kc in range(8):
                kb.op(PE, lambda kc=kc, s=s, b=b: te.matmul(psb(b, 0, MEM), ring[:, s, kc * 128:(kc + 1) * 128],
                                                           memTb[:, kc, :], start=(kc == 0), stop=(kc == 7)),
                      R=[R_ring[s]] + R_xT, W=[R_ps[b]])
            kb.op(ACT, lambda b=b, key=key: se.activation(out=kmT[:, key, :], in_=psb(b, 0, MEM), func=AF.Copy),
                  R=[R_ps[b]], W=[R_km])
        else:
            blk, hk = key
            if hk == 1:
                s0 = (i - 1) % NSLOT
                for mc in range(2):
                    b = next_pa()
                    for kc in range(8):
                        ss = s0 if kc < 4 else s
                        kb.op(PE, lambda kc=kc, ss=ss, b=b, mc=mc: te.matmul(
                            psb(b), memTb[:, kc, mc * 128:(mc + 1) * 128],
                            ring[:, ss, (kc % 4) * 512:(kc % 4 + 1) * 512], start=(kc == 0), stop=(kc == 7)),
                            R=[R_ring[s0], R_ring[s]] + R_xT, W=[R_ps[b]])
                    kb.op(ACT, lambda b=b, mc=mc, blk=blk: se.activation(
                        out=vm[:, mc, blk * 512:(blk + 1) * 512], in_=psb(b), func=AF.Copy),
                        R=[R_ps[b]], W=[R_vm])

    bnst4 = sb("bnst4", [128, 4, 12], F32)
    bnmv4 = sb("bnmv4", [128, 4, 2], F32)
    R_bn4 = [_Reg(f"bn4_{c}") for c in range(4)]
    R_sm4 = [_Reg(f"sm4_{c}") for c in range(4)]

    def layer_norm_all(gname, bname, make_T, after=None):
        for c in range(4):
            for hlf in range(2):
                kb.op(DVE, lambda c=c, hlf=hlf: ve.bn_stats(out=bnst4[:, c, hlf * 6:(hlf + 1) * 6],
                                                            in_=XB[:, c, hlf * 512:(hlf + 1) * 512]),
                      R=[R_XB[c]], W=[R_bn4[c]])
            kb.op(DVE, lambda c=c: ve.bn_aggr(out=bnmv4[:, c, :], in_=bnst4[:, c, :]), R=[R_bn4[c]], W=[R_bn4[c]])
            kb.op(DVE, lambda c=c: ve.tensor_scalar(out=sm[:, 48 + 2 * c:49 + 2 * c], in0=bnmv4[:, c, 1:2], scalar1=EPS,
                                                    scalar2=None, op0=ALU.add), R=[R_bn4[c]], W=[R_sm4[c]])
        for c in range(4):
            kb.op(ACT, lambda c=c: se.activation(out=sm[:, 48 + 2 * c:49 + 2 * c], in_=sm[:, 48 + 2 * c:49 + 2 * c],
                                                 func=AF.Ln), R=[R_sm4[c]], W=[R_sm4[c]])
            kb.op(ACT, lambda c=c: se.activation(out=sm[:, 48 + 2 * c:49 + 2 * c], in_=sm[:, 48 + 2 * c:49 + 2 * c],
                                                 func=AF.Exp, scale=-0.5), R=[R_sm4[c]], W=[R_sm4[c]])
        for c in range(4):
            kb.op(DVE, lambda c=c: ve.scalar_tensor_tensor(out=sm[:, 49 + 2 * c:50 + 2 * c], in0=bnmv4[:, c, 0:1], scalar=-1.0,
                                                           in1=sm[:, 48 + 2 * c:49 + 2 * c], op0=ALU.mult, op1=ALU.mult),
                  R=[R_sm4[c], R_bn4[c]], W=[R_sm4[c]])
        for c in range(4):
            kb.op(ACT, lambda c=c: se.activation(out=XB[:, c, :], in_=XB[:, c, :], func=AF.Identity,
                                                 bias=sm[:, 49 + 2 * c:50 + 2 * c], scale=sm[:, 48 + 2 * c:49 + 2 * c]),
                  R=[R_XB[c], R_sm4[c]], W=[R_XB[c]])
        for c in range(4):
            kb.op(DVE, lambda c=c: ve.tensor_tensor(out=XB[:, c, :], in0=XB[:, c, :], in1=C(gname), op=ALU.mult),
                  R=[R_XB[c], R_cst], W=[R_XB[c]])
            kb.op(DVE, lambda c=c: ve.tensor_tensor(out=XB[:, c, :], in0=XB[:, c, :], in1=C(bname), op=ALU.add),
                  R=[R_XB[c], R_cst], W=[R_XB[c]])
            if after is not None:
                after(c)
        if make_T:
            for c in range(4):
                Hc, RHc, bk = ((H1, R_H1, 5), (H2, R_H2, 6))[c % 2]
                kb.op(ACT, lambda c=c, Hc=Hc: se.activation(out=Hc[:, :], in_=XB[:, c, :], func=AF.Copy),
                      R=[R_XB[c]], W=[RHc])
                for j in range(8):
                    kb.op(PE, lambda j=j, Hc=Hc, bk=bk: te.transpose(out=psbf(bk)[:, j * 128:(j + 1) * 128],
                                                                     in_=Hc[:, j * 128:(j + 1) * 128],
                                                                     identity=ident_bf[:, :]),
                          R=[RHc, R_c2], W=[R_ps[bk]])
                kb.op(ACT, lambda c=c, bk=bk: se.activation(out=xTb[:, :, c * 128:(c + 1) * 128],
                                                           in_=psbf(bk).rearrange("p (j t) -> p j t", j=8), func=AF.Copy),
                      R=[R_ps[bk]], W=[R_xT[c]])

    def proj_tm_accum(kind, nq, lhs_of, lhs_regs, half):
        for q in range(nq):
            sl, rg = unit(kind, (half, q))
            for c in range(4):
                for k4 in range(4):
                    kc = q * 4 + k4
                    kb.op(PE, lambda c=c, kc=kc, k4=k4, sl=sl: te.matmul(
                        psb(c), lhs_of(kc, c), sl[:, k4 * 512:(k4 + 1) * 512],
                        start=(kc == 0), stop=(kc == nq * 4 - 1)),
                        R=[rg] + lhs_regs(kc, c), W=[R_ps[c]])

    def residual_from_psum(half):
        for c in range(4):
            kb.op(DVE, lambda c=c: ve.scalar_tensor_tensor(
                out=XB[:, c, half * 512:(half + 1) * 512], in0=XB[:, c, half * 512:(half + 1) * 512],
                scalar=ALPHA, in1=psb(c), op0=ALU.mult, op1=ALU.add),
                R=[R_XB[c], R_ps[c]], W=[R_XB[c]])

    def chk(name):
        if stop == name:
            raise _Stop()

    def record(body):
        rec = []
        real_op, real_dma = kb.op, kb.dma
        kb.op = lambda E, fn, R=(), W=(): rec.append(lambda: real_op(E, fn, list(R), list(W)))
        kb.dma = lambda Q, sn, out, in_, R=(), W=(): rec.append(lambda: real_dma(Q, sn, out, in_, list(R), list(W)))
        try:
            body()
        finally:
            del kb.op
            del kb.dma
        return rec

    def merge(la, lb):
        out, i, j = [], 0, 0
        while i < len(la) or j < len(lb):
            if j >= len(lb) or (i < len(la) and i * len(lb) <= j * len(la)):
                out.append(la[i]); i += 1
            else:
                out.append(lb[j]); j += 1
        return out

    def pipelined(items, phase_a, phase_b):
        prev = None
        for it in items:
            ctx = phase_a(it)
            if prev is not None:
                phase_b(*prev)
            prev = (it, ctx)
        phase_b(*prev)

    try:
      chk("s0")
      for t in range(NT):
          t0 = t * T
          def prologue(tt):
              kb.op(DVE, lambda: ve.memset(fence[:, 0:1], 0.0), R=view_b, W=view_a + [R_fence])
              kb.dma(POOL, "xT", xTb[:, :, :], xT_d[:, :, tt * T:(tt + 1) * T], W=R_xT)
              kb.dma(POOL, "cs", cs_sb[:, :], cs_d[tt, :, :], W=[R_cs])

          if t == 0:
              prologue(0)
          for c in range(4):
              kb.dma(POOL, f"xb{c}", XB[:, c, :], xn_d[t0 + c * 128:t0 + (c + 1) * 128, :], W=[R_XB[c]])

          def xbc_a(j):
              sl, rg = unit("xbc", j, hold=1)
              b = next_pa()
              sg = j % 2
              for kc in range(8):
                  kb.op(PE, lambda kc=kc, b=b, sl=sl: te.matmul(psb(b), sl[:, kc * 128:(kc + 1) * 128], xTb[:, kc, :],
                                                                start=(kc == 0), stop=(kc == 7)),
                        R=[rg] + R_xT, W=[R_ps[b]])
              kb.op(DVE, lambda j=j, sg=sg: ve.tensor_copy(out=stg[:, sg, 0:3], in_=halo_s[:, j, 0:3]),
                    R=[R_hs[j]], W=[R_stg[sg]])
              kb.op(ACT, lambda b=b, sg=sg: se.activation(out=stg[:, sg, 3:515], in_=psb(b), func=AF.Copy),
                    R=[R_ps[b]], W=[R_stg[sg]])
              kb.op(DVE, lambda j=j, sg=sg: ve.tensor_copy(out=halo_s[:, j, 0:3], in_=stg[:, sg, 512:515]),
                    R=[R_stg[sg]], W=[R_hs[j]])
              return sl, rg, b, sg

          def xbc_b(j, ctx):
              sl, rg, b, sg = ctx
              b2 = 2 + b
              for k in range(4):
                  kb.op(PE, lambda k=k, b2=b2, sl=sl, sg=sg: te.matmul(
                      psb(b2), sl[:, 1024 + k * 128:1024 + (k + 1) * 128], stg[:, sg, k:k + 512],
                      start=(k == 0), stop=(k == 3)), R=[rg, R_stg[sg]], W=[R_ps[b2]])
              kb.op(ACT, lambda j=j, b2=b2: se.activation(out=xbcT[:, j, :], in_=psb(b2), func=AF.Silu,
                                                           bias=C("scb", j, j + 1)),
                    R=[R_ps[b2], R_cst], W=[R_xbc[j]])

          if t == 0:
              pipelined(range(12), xbc_a, xbc_b)
          chk("s1a")
          def qk_a(j):
              sl, rg = unit("qk", j)
              b = next_pa()
              sg = j % 2
              for kc in range(8):
                  kb.op(PE, lambda kc=kc, b=b, sl=sl: te.matmul(psb(b), sl[:, kc * 128:(kc + 1) * 128], xTb[:, kc, :],
                                                                start=(kc == 0), stop=(kc == 7)),
                        R=[rg] + R_xT, W=[R_ps[b]])
              kb.op(ACT, lambda b=b, sg=sg: se.activation(out=raw[:, sg, :], in_=psb(b), func=AF.Copy),
                    R=[R_ps[b]], W=[R_raw[sg]])
              return b, sg

          def qk_b(j, ctx):
              b, sg = ctx
              b2 = 2 + b
              kb.op(PE, lambda b2=b2, sg=sg: te.matmul(psb(b2), prot_bf[:, :], raw[:, sg, :], start=True, stop=True),
                    R=[R_c2, R_raw[sg]], W=[R_ps[b2]])
              kb.op(DVE, lambda b=b: ve.tensor_tensor(out=rt1[:, :], in0=psb(b), in1=cs_sb[:, 0:512], op=ALU.mult),
                    R=[R_ps[b], R_cs], W=[R_rt1])
              kb.op(DVE, lambda b2=b2: ve.tensor_tensor(out=rt2[:, :], in0=psb(b2), in1=cs_sb[:, 512:1024], op=ALU.mult),
                    R=[R_ps[b2], R_cs], W=[R_rt2])
              kb.op(DVE, lambda j=j: ve.tensor_tensor(out=qkT[:, j, :], in0=rt1[:, :], in1=rt2[:, :], op=ALU.add),
                    R=[R_rt1, R_rt2], W=[R_qk[j]])
              if j < 4:
                  kb.op(DVE, lambda j=j: ve.tensor_tensor(
                      out=qsT[:, j, :].rearrange("p (c n) -> p c n", c=4),
                      in0=qkT[:, j, :].rearrange("p (c n) -> p c n", c=4),
                      in1=C("qdecT", j * 128, (j + 1) * 128).unsqueeze(1).broadcast_to([128, 4, 128]), op=ALU.mult),
                      R=[R_qk[j], R_cst], W=[R_qs[j]])

          pipelined(range(8), qk_a, qk_b)
          chk("s1b")
          for j in range(8):
              sl, rg = unit("g", j)
              b = next_pa()
              for kc in range(8):
                  kb.op(PE, lambda kc=kc, b=b, sl=sl: te.matmul(psb(b), sl[:, kc * 128:(kc + 1) * 128], xTb[:, kc, :],
                                                                start=(kc == 0), stop=(kc == 7)),
                        R=[rg] + R_xT, W=[R_ps[b]])
              kb.op(ACT, lambda j=j, b=b: se.activation(out=gT[:, j, :], in_=psb(b), func=AF.Silu),
                    R=[R_ps[b]], W=[R_g[j]])
          chk("s1c")
          for kind, dst, regs, fn in (("z", zs, R_zs, AF.Silu), ("v", vt, R_v, AF.Copy)):
              for blk in range(2):
                  proj_tm_accum(kind, 2, lambda kc, c: xTb[:, kc, c * 128:(c + 1) * 128], lambda kc, c: [R_xT[c]], blk)
                  for c in range(4):
                      kb.op(ACT, lambda c=c, blk=blk, dst=dst, fn=fn: se.activation(
                          out=dst[:, c, blk * 512:(blk + 1) * 512], in_=psb(c), func=fn),
                          R=[R_ps[c]], W=[regs[c]])
          chk("s1d")
          sl, rg = unit("dt", 0)
          for c in range(4):
              for kc in range(8):
                  kb.op(PE, lambda kc=kc, c=c, sl=sl: te.matmul(
                      psb(4, c * 16, (c + 1) * 16), xTb[:, kc, c * 128:(c + 1) * 128], sl[:, kc * 16:(kc + 1) * 16],
                      start=(kc == 0), stop=(kc == 7)), R=[rg, R_xT[c]], W=[R_ps[4]])
          kb.op(DVE, lambda: ve.tensor_tensor(out=dt_tmp[:, :, :], in0=psb(4, 0, 64).rearrange("p (c h) -> p c h", c=4),
                                              in1=C("dtb").unsqueeze(1).broadcast_to([128, 4, 16]), op=ALU.add),
                R=[R_ps[4], R_cst], W=[R_dtt])
          kb.op(ACT, lambda: se.activation(out=dt_tmp[:, :, :], in_=dt_tmp[:, :, :], func=AF.Exp), R=[R_dtt], W=[R_dtt])
          kb.op(ACT, lambda: se.activation(out=dt_all[:, :, :], in_=dt_tmp[:, :, :], func=AF.Ln, bias=1.0),
                R=[R_dtt], W=[R_dt])

          chk("s1")
          def ssd_body(c):
              cs_ = slice(c * 128, (c + 1) * 128)
              kb.op(DVE, lambda: ve.tensor_tensor(out=a_sb[:, :], in0=dt_all[:, c, :], in1=arow[:, :], op=ALU.mult),
                    R=[R_dt, R_c2], W=[R_a])
              for g in range(2):
                  kb.op(DVE, lambda g=g: ve.tensor_tensor(
                      out=rhs_a[:, g * 8:(g + 1) * 8, :], in0=C("tri").unsqueeze(1).broadcast_to([128, 8, 128]),
                      in1=a_sb[:, g * 8:(g + 1) * 8].unsqueeze(2).broadcast_to([128, 8, 128]), op=ALU.mult),
                      R=[R_a, R_cst], W=[R_rhsa[g]])
                  for q in range(2):
                      b = g * 2 + q
                      kb.op(PE, lambda b=b, g=g, q=q: te.matmul(
                          psb(b), C("strict"),
                          rhs_a[:, g * 8 + q * 4:g * 8 + q * 4 + 4, :].rearrange("p h l -> p (h l)"),
                          start=True, stop=True), R=[R_cst, R_rhsa[g]], W=[R_ps[b]])
                  kb.op(ACT, lambda g=g: se.activation(out=segT[:, g * 8:(g + 1) * 8, :].rearrange("p h l -> p (h l)"),
                                                       in_=ps2(2 * g), func=AF.Exp),
                        R=[R_ps[2 * g], R_ps[2 * g + 1]], W=[R_seg[g]])
              for g in range(2):
                  kb.op(PE, lambda g=g: te.matmul(psb(4, g * 128, (g + 1) * 128), xbcT[:, 8 + g, cs_], xbcT[:, 10 + g, cs_],
                                                  start=True, stop=True),
                        R=[R_xbc[8 + g], R_xbc[10 + g]], W=[R_ps[4]])
              kb.op(DVE, lambda: ve.tensor_tensor(out=cbm[:, :, :], in0=psb(4, 0, 256).rearrange("p (g l) -> p g l", g=2),
                                                  in1=C("tri").unsqueeze(1).broadcast_to([128, 2, 128]), op=ALU.mult),
                    R=[R_ps[4], R_cst], W=[R_cbm])
              for g in range(2):
                  kb.op(DVE, lambda g=g: ve.tensor_tensor(
                      out=attT[:, g * 8:(g + 1) * 8, :], in0=segT[:, g * 8:(g + 1) * 8, :],
                      in1=cbm[:, g, :].unsqueeze(1).broadcast_to([128, 8, 128]), op=ALU.mult),
                      R=[R_seg[g], R_cbm], W=[R_att[g]])
              for j in range(8):
                  kb.op(PE, lambda j=j: te.transpose(out=psbf(5)[:, j * 128:(j + 1) * 128], in_=xbcT[:, j, cs_],
                                                     identity=ident_bf[:, :]),
                        R=[R_xbc[j], R_c2], W=[R_ps[5]])
              kb.op(DVE, lambda: ve.tensor_tensor(out=dtd[:, :], in0=dt_all[:, c, :], in1=segT[:, :, 127], op=ALU.mult),
                    R=[R_dt] + R_seg, W=[R_dtd])
              x3 = psbf(5).rearrange("p (h d) -> p h d", h=16)
              kb.op(DVE, lambda: ve.tensor_tensor(out=xdt[:, :].rearrange("p (h d) -> p h d", h=16), in0=x3,
                                                  in1=dt_all[:, c, :].unsqueeze(2).broadcast_to([128, 16, 64]),
                                                  op=ALU.mult), R=[R_ps[5], R_dt], W=[R_xdt])
              kb.op(DVE, lambda: ve.tensor_tensor(out=xdtd[:, :].rearrange("p (h d) -> p h d", h=16), in0=x3,
                                                  in1=dtd[:, :].unsqueeze(2).broadcast_to([128, 16, 64]),
                                                  op=ALU.mult), R=[R_ps[5], R_dtd], W=[R_xdtd])
              kb.op(DVE, lambda: ve.tensor_tensor(out=F1[:, :].rearrange("p (h d) -> p h d", h=16), in0=x3,
                                                  in1=C("drow").unsqueeze(2).broadcast_to([128, 16, 64]),
                                                  op=ALU.mult), R=[R_ps[5], R_cst], W=[R_F1])
              for g in range(2):
                  kb.op(PE, lambda g=g: te.transpose(out=psbf(4)[:, 512 + g * 128:512 + (g + 1) * 128],
                                                     in_=xbcT[:, 8 + g, cs_], identity=ident_bf[:, :]),
                        R=[R_xbc[8 + g], R_c2], W=[R_ps[4]])
              kb.op(ACT, lambda: se.activation(out=Btok[:, :, :].rearrange("p g n -> p (g n)"),
                                               in_=psbf(4)[:, 512:768], func=AF.Copy), R=[R_ps[4]], W=[R_Btok])
              kb.op(PE, lambda: te.matmul(psb(4, 384, 400), C("tri"), a_sb[:, :], start=True, stop=True),
                    R=[R_cst, R_a], W=[R_ps[4]])
              kb.op(PE, lambda: te.matmul(psb(4, 400, 416), C("ones"), a_sb[:, :], start=True, stop=True),
                    R=[R_cst, R_a], W=[R_ps[4]])
              kb.op(ACT, lambda: se.activation(out=dfcd[:, :], in_=psb(4, 384, 416), func=AF.Exp),
                    R=[R_ps[4]], W=[R_dfcd])
              for h in range(16):
                  kb.op(PE, lambda h=h: te.matmul(ps2(0)[:, h * 64:(h + 1) * 64], attT[:, h, :],
                                                  xdt[:, h * 64:(h + 1) * 64], start=True, stop=True),
                        R=[R_att[h // 8], R_xdt], W=[R_ps[h // 8]])
              for g in range(2):
                  kb.op(PE, lambda g=g: te.matmul(psb(2 + g), xbcT[:, 10 + g, cs_], hT_bf[:, g * 512:(g + 1) * 512],
                                                  start=True, stop=True),
                        R=[R_xbc[10 + g], R_hTb], W=[R_ps[2 + g]])
              kb.op(DVE, lambda: ve.tensor_tensor(out=F2[:, :].rearrange("p (h d) -> p h d", h=16),
                                                  in0=ps2(2).rearrange("p (h d) -> p h d", h=16),
                                                  in1=dfcd[:, 0:16].unsqueeze(2).broadcast_to([128, 16, 64]), op=ALU.mult),
                    R=[R_ps[2], R_ps[3], R_dfcd], W=[R_F2])
              kb.op(POOL, lambda: ge.tensor_tensor(out=F1[:, :], in0=F1[:, :], in1=F2[:, :], op=ALU.add),
                    R=[R_F1, R_F2], W=[R_F1])
              kb.op(DVE, lambda: ve.tensor_tensor(out=F1[:, :], in0=ps2(0), in1=F1[:, :], op=ALU.add),
                    R=[R_ps[0], R_ps[1], R_F1], W=[R_F1])
              kb.op(POOL, lambda: ge.tensor_tensor(out=F1[:, :], in0=F1[:, :], in1=zs[:, c, :], op=ALU.mult),
                    R=[R_F1, R_zs[c]], W=[R_F1])
              for g in range(2):
                  kb.op(ACT, lambda g=g: se.activation(out=asil[:, :], in_=F1[:, g * 512:(g + 1) * 512], func=AF.Square,
                                                       accum_out=sm[:, 8 + g:9 + g]), R=[R_F1], W=[R_asil, R_smS])
              kb.op(DVE, lambda: ve.tensor_scalar(out=sm[:, 8:10], in0=sm[:, 8:10], scalar1=1.0 / 512.0, scalar2=EPS,
                                                  op0=ALU.mult, op1=ALU.add), R=[R_smS], W=[R_smS])
              kb.op(ACT, lambda: se.activation(out=sm[:, 8:10], in_=sm[:, 8:10], func=AF.Ln), R=[R_smS], W=[R_smS])
              kb.op(ACT, lambda: se.activation(out=sm[:, 8:10], in_=sm[:, 8:10], func=AF.Exp, scale=-0.5),
                    R=[R_smS], W=[R_smS])
              for g in range(2):
                  kb.op(DVE, lambda g=g: ve.tensor_scalar(out=H1[:, g * 512:(g + 1) * 512], in0=F1[:, g * 512:(g + 1) * 512],
                                                          scalar1=sm[:, 8 + g:9 + g], scalar2=None, op0=ALU.mult),
                        R=[R_F1, R_smS], W=[R_H1])
              for j in range(8):
                  kb.op(PE, lambda j=j: te.transpose(out=psbf(5)[:, j * 128:(j + 1) * 128], in_=H1[:, j * 128:(j + 1) * 128],
                                                     identity=ident_bf[:, :]), R=[R_H1, R_c2], W=[R_ps[5]])
              kb.op(DVE, lambda: ve.tensor_tensor(out=ymixT[:, 0:8, cs_], in0=psbf(5).rearrange("p (j t) -> p j t", j=8),
                                                  in1=C("normw").unsqueeze(2).broadcast_to([128, 8, 128]), op=ALU.mult),
                    R=[R_ps[5], R_cst], W=[R_ym[c]])
              for g in range(2):
                  kb.op(PE, lambda g=g: te.matmul(psb(2 + g), Btok[:, g, :], xdtd[:, g * 512:(g + 1) * 512],
                                                  start=True, stop=True), R=[R_Btok, R_xdtd], W=[R_ps[2 + g]])
              kb.op(POOL, lambda: ge.tensor_tensor(out=hT[:, :].rearrange("p (h d) -> p h d", h=16),
                                                  in0=hT[:, :].rearrange("p (h d) -> p h d", h=16),
                                                  in1=dfcd[:, 16:32].unsqueeze(2).broadcast_to([128, 16, 64]),
                                                  op=ALU.mult), R=[R_hT, R_dfcd], W=[R_hT])
              kb.op(DVE, lambda: ve.tensor_tensor(out=hT[:, :], in0=ps2(2), in1=hT[:, :], op=ALU.add),
                    R=[R_hT, R_ps[2], R_ps[3]], W=[R_hT])
              kb.op(ACT, lambda: se.activation(out=hT_bf[:, :], in_=hT[:, :], func=AF.Copy), R=[R_hT], W=[R_hTb])

          def ret_body(c):
              cs_ = slice(c * 128, (c + 1) * 128)
              for j in range(4):
                  kb.op(PE, lambda j=j: te.transpose(out=psbf(6)[:, j * 128:(j + 1) * 128], in_=qkT[:, 4 + j, cs_],
                                                     identity=ident_bf[:, :]), R=[R_qk[4 + j], R_c2], W=[R_ps[6]])
              kb.op(DVE, lambda: ve.tensor_tensor(out=kd[:, :].rearrange("p (h d) -> p h d", h=8),
                                                  in0=psbf(6)[:, 0:512].rearrange("p (h d) -> p h d", h=8),
                                                  in1=C("kdec").unsqueeze(2).broadcast_to([128, 8, 64]), op=ALU.mult),
                    R=[R_ps[6], R_cst], W=[R_kd])
              for h in range(8):
                  p0 = (h % 2) * 64
                  kb.op(PE, lambda h=h, p0=p0: te.matmul(ps[p0:p0 + 64, 7, (h // 2) * 128:(h // 2 + 1) * 128],
                                                         kd[:, h * 64:(h + 1) * 64], vt[:, c, h * 128:(h + 1) * 128],
                                                         start=True, stop=True), R=[R_kd, R_v[c]], W=[R_ps[7]])
              kb.op(POOL, lambda: ge.tensor_tensor(out=rS[:, :].rearrange("p (j e) -> p j e", j=4),
                                                  in0=rS[:, :].rearrange("p (j e) -> p j e", j=4),
                                                  in1=C("cdr").unsqueeze(2).broadcast_to([128, 4, 128]), op=ALU.mult),
                    R=[R_rS, R_cst], W=[R_rS])
              kb.op(DVE, lambda: ve.tensor_tensor(out=rS[:, :], in0=psb(7), in1=rS[:, :], op=ALU.add),
                    R=[R_rS, R_ps[7]], W=[R_rS])
              for h in range(8):
                  p0 = (h % 2) * 64
                  kb.op(PE, lambda h=h, p0=p0: te.matmul(psb(6 + h % 2, (h // 2) * 128, (h // 2 + 1) * 128),
                                                         qkT[p0:p0 + 64, 4 + h // 2, cs_],
                                                         qkT[p0:p0 + 64, h // 2, cs_], start=True, stop=True),
                        R=[R_qk[4 + h // 2], R_qk[h // 2]], W=[R_ps[6 + h % 2]])
              kb.op(DVE, lambda: ve.tensor_tensor(
                  out=sT[:, :, :].rearrange("p (j par) n -> p par j n", par=2),
                  in0=ps[:, 6:8, :].rearrange("p par (j n) -> p par j n", j=4),
                  in1=C("decT").rearrange("p (j par n) -> p par j n", j=4, par=2), op=ALU.mult),
                    R=[R_ps[6], R_ps[7], R_cst], W=[R_sT])
              for h in range(8):
                  p0 = (h % 2) * 64
                  kb.op(PE, lambda h=h: te.matmul(ps2(6)[:, h * 128:(h + 1) * 128], sT[:, h, :],
                                                  vt[:, c, h * 128:(h + 1) * 128], start=True, stop=False),
                        R=[R_sT, R_v[c]], W=[R_ps[6 + h // 4]])
                  kb.op(PE, lambda h=h, p0=p0: te.matmul(ps2(6)[:, h * 128:(h + 1) * 128], qsT[p0:p0 + 64, h // 2, cs_],
                                                         rS_bf[p0:p0 + 64, (h // 2) * 128:(h // 2 + 1) * 128],
                                                         start=False, stop=True),
                        R=[R_qs[h // 2], R_rSb], W=[R_ps[6 + h // 4]])
              kb.op(ACT, lambda: se.activation(out=rS_bf[:, :], in_=rS[:, :], func=AF.Copy), R=[R_rS], W=[R_rSb])
              y3 = ps2(6).rearrange("p (h e) -> p h e", h=8)
              kb.op(DVE, lambda: ve.tensor_reduce(out=sm[:, 16:24], in_=y3, axis=AX.X, op=ALU.add),
                    R=[R_ps[6], R_ps[7]], W=[R_smR])
              kb.op(ACT, lambda: se.activation(out=pT[:, :, :].rearrange("p h e -> p (h e)"), in_=ps2(6), func=AF.Square),
                    R=[R_ps[6], R_ps[7]], W=[R_pT])
              kb.op(DVE, lambda: ve.tensor_reduce(out=sm[:, 24:32], in_=pT[:, :, :],
                                                  axis=AX.X, op=ALU.add), R=[R_pT], W=[R_smR])
              kb.op(DVE, lambda: ve.tensor_scalar(out=sm[:, 16:24], in0=sm[:, 16:24], scalar1=1.0 / 128.0, scalar2=None,
                                                  op0=ALU.mult), R=[R_smR], W=[R_smR])
              kb.op(DVE, lambda: ve.tensor_tensor(out=sm[:, 32:40], in0=sm[:, 16:24], in1=sm[:, 16:24], op=ALU.mult),
                    R=[R_smR], W=[R_smR])
              kb.op(DVE, lambda: ve.scalar_tensor_tensor(out=sm[:, 24:32], in0=sm[:, 24:32], scalar=1.0 / 128.0,
                                                         in1=sm[:, 32:40], op0=ALU.mult, op1=ALU.subtract),
                    R=[R_smR], W=[R_smR])
              kb.op(DVE, lambda: ve.tensor_scalar(out=sm[:, 24:32], in0=sm[:, 24:32], scalar1=EPS, scalar2=None,
                                                  op0=ALU.add), R=[R_smR], W=[R_smR])
              kb.op(ACT, lambda: se.activation(out=sm[:, 24:32], in_=sm[:, 24:32], func=AF.Ln), R=[R_smR], W=[R_smR])
              kb.op(ACT, lambda: se.activation(out=sm[:, 24:32], in_=sm[:, 24:32], func=AF.Exp, scale=-0.5),
                    R=[R_smR], W=[R_smR])
              kb.op(DVE, lambda: ve.scalar_tensor_tensor(out=sm[:, 32:40], in0=sm[:, 16:24], scalar=-1.0,
                                                         in1=sm[:, 24:32], op0=ALU.mult, op1=ALU.mult),
                    R=[R_smR], W=[R_smR])
              for h in range(8):
                  kb.op(ACT, lambda h=h: se.activation(out=H2[:, h * 128:(h + 1) * 128], in_=ps2(6)[:, h * 128:(h + 1) * 128],
                                                       func=AF.Identity, bias=sm[:, 32 + h:33 + h],
                                                       scale=sm[:, 24 + h:25 + h]),
                        R=[R_ps[6 + h // 4], R_smR], W=[R_H2])
              for h in range(8):
                  kb.op(PE, lambda h=h: te.transpose(out=psbf(6)[:, h * 128:(h + 1) * 128], in_=H2[:, h * 128:(h + 1) * 128],
                                                     identity=ident_bf[:, :]), R=[R_H2, R_c2], W=[R_ps[6]])
              for j in range(8):
                  kb.op(ACT, lambda j=j: se.activation(out=pT[:, j, :], in_=psbf(6)[:, j * 128:(j + 1) * 128],
                                                       func=AF.Identity, bias=C("gnb", j, j + 1),
                                                       scale=C("gnw", j, j + 1)),
                        R=[R_ps[6], R_cst], W=[R_pT])
              kb.op(DVE, lambda: ve.tensor_tensor(out=ymixT[:, 8:16, cs_], in0=pT[:, :, :], in1=gT[:, :, cs_], op=ALU.mult),
                    R=[R_pT] + R_g, W=[R_ym[c]])

          for c in range(4):
              la = record(lambda: ssd_body(c))
              lb = record(lambda: ret_body(c))
              for th in merge(la, lb):
                  th()

          if dbg and t == 0:
              kb.dma(SP, "dbg_ymix", dbg_d["ymix"], ymixT[:, :, :].rearrange("p j t -> p (j t)"), R=R_ym)

          chk("s2")
          for half in range(2):
              proj_tm_accum("wout", 4, lambda kc, c: ymixT[:, kc, c * 128:(c + 1) * 128], lambda kc, c: [R_ym[c]], half)
              residual_from_psum(half)
          layer_norm_all("ln1g", "ln1b", True)
          if dbg and t == 0:
              kb.dma(SP, "dbg_x1", dbg_d["x1"], XB[:, :, :].rearrange("p c n -> p (c n)"), R=R_XB)

          chk("s3")
          kb.op(DVE, lambda: ve.memset(fence[:, 0:1], 0.0), R=view_a, W=view_b + [R_fence])

          for j in range(8):
              sl, rg = unit("xq", j)
              b = next_pa()
              for kc in range(8):
                  kb.op(PE, lambda kc=kc, b=b, sl=sl: te.matmul(psb(b), sl[:, kc * 128:(kc + 1) * 128], xTb[:, kc, :],
                                                                start=(kc == 0), stop=(kc == 7)),
                        R=[rg] + R_xT, W=[R_ps[b]])
              kb.op(ACT, lambda j=j, b=b: se.activation(out=qxT[:, j, :], in_=psb(b), func=AF.Copy),
                    R=[R_ps[b]], W=[R_qx[j]])
          def xattn_body(c, S):
              cs_ = slice(c * 128, (c + 1) * 128)
              b0, bt, Fp, R_Fp, Hp, R_Hp, Tp, R_Tp, so, R_so = S
              sc = ps[:, b0:b0 + 2, :].rearrange("p a b -> p (a b)")
              for h in range(4):
                  for dc in range(2):
                      kb.op(PE, lambda h=h, dc=dc: te.matmul(sc[:, h * 256:(h + 1) * 256], qxT[:, h * 2 + dc, cs_],
                                                             kmT[:, h * 2 + dc, :], start=(dc == 0), stop=(dc == 1)),
                            R=[R_qx[h * 2 + dc], R_km], W=[R_ps[b0 + h // 2]])
              kb.op(DVE, lambda: ve.tensor_reduce(out=sm[:, so:so + 4], in_=sc.rearrange("p (h m) -> p h m", h=4),
                                                  axis=AX.X, op=ALU.max), R=[R_ps[b0], R_ps[b0 + 1]], W=[R_so])
              kb.op(DVE, lambda: ve.tensor_scalar(out=sm[:, so:so + 4], in0=sm[:, so:so + 4], scalar1=-1.0 / 16.0,
                                                  scalar2=None, op0=ALU.mult), R=[R_so], W=[R_so])
              for h in range(4):
                  kb.op(ACT, lambda h=h: se.activation(out=Fp[:, h * 256:(h + 1) * 256], in_=sc[:, h * 256:(h + 1) * 256],
                                                       func=AF.Exp, bias=sm[:, so + h:so + h + 1], scale=1.0 / 16.0,
                                                       accum_out=sm[:, so + 4 + h:so + 5 + h]),
                        R=[R_ps[b0 + h // 2], R_so], W=[R_Fp, R_so])
              kb.op(DVE, lambda: ve.reciprocal(out=sm[:, so + 4:so + 8], in_=sm[:, so + 4:so + 8]), R=[R_so], W=[R_so])
              for h in range(4):
                  kb.op(DVE, lambda h=h: ve.tensor_scalar(out=Hp[:, h * 256:(h + 1) * 256], in0=Fp[:, h * 256:(h + 1) * 256],
                                                          scalar1=sm[:, so + 4 + h:so + 5 + h], scalar2=None, op0=ALU.mult),
                        R=[R_Fp, R_so], W=[R_Hp])
              for i in range(8):
                  kb.op(PE, lambda i=i: te.transpose(out=psbf(bt)[:, i * 128:(i + 1) * 128], in_=Hp[:, i * 128:(i + 1) * 128],
                                                     identity=ident_bf[:, :]), R=[R_Hp, R_c2], W=[R_ps[bt]])
              kb.op(ACT, lambda: se.activation(out=Tp[:, :, :].rearrange("p i l -> p (i l)"), in_=psbf(bt), func=AF.Copy),
                    R=[R_ps[bt]], W=[R_Tp])
              for h in range(4):
                  for dch in range(2):
                      i = h * 2 + dch
                      for mc in range(2):
                          kb.op(PE, lambda h=h, dch=dch, mc=mc, i=i: te.matmul(
                              sc[:, i * 128:(i + 1) * 128], vm[:, mc, h * 256 + dch * 128:h * 256 + (dch + 1) * 128],
                              Tp[:, h * 2 + mc, :], start=(mc == 0), stop=(mc == 1)),
                              R=[R_vm, R_Tp], W=[R_ps[b0 + i // 4]])
              kb.op(ACT, lambda: se.activation(out=oT[:, :, cs_], in_=sc.rearrange("p (i l) -> p i l", i=8),
                                               func=AF.Copy), R=[R_ps[b0], R_ps[b0 + 1]], W=[R_oT[c]])

          S_A = (0, 2, F2, R_F2, H2, R_H2, pT, R_pT, 40, R_sm)
          S_B = (3, 5, F1, R_F1, H1, R_H1, sT, R_sT, 56, R_smS)
          for pr in range(2):
              la = record(lambda: xattn_body(2 * pr, S_A))
              lb = record(lambda: xattn_body(2 * pr + 1, S_B))
              for th in merge(la, lb):
                  th()
          for half in range(2):
              proj_tm_accum("xo", 2, lambda kc, c: oT[:, kc, c * 128:(c + 1) * 128], lambda kc, c: [R_oT[c]], half)
              residual_from_psum(half)
          layer_norm_all("ln2g", "ln2b", True)
          if dbg and t == 0:
              kb.dma(SP, "dbg_x2", dbg_d["x2"], XB[:, :, :].rearrange("p c n -> p (c n)"), R=R_XB)

          chk("s4")
          def ffn_a(it):
              j, part = it
              jj = part * 22 + j
              sl, rg = unit(("fa", "fu")[part], j, hold=1)
              b = next_pa()
              sg = part
              for kc in range(8):
                  kb.op(PE, lambda kc=kc, b=b, sl=sl: te.matmul(psb(b), sl[:, kc * 128:(kc + 1) * 128], xTb[:, kc, :],
                                                                start=(kc == 0), stop=(kc == 7)),
                        R=[rg] + R_xT, W=[R_ps[b]])
              kb.op(DVE, lambda jj=jj, sg=sg: ve.tensor_copy(out=stg[:, sg, 0:2], in_=halo_f[:, jj, 0:2]),
                    R=[R_hf[jj]], W=[R_stg[sg]])
              kb.op(ACT, lambda b=b, sg=sg, jj=jj: se.activation(out=stg[:, sg, 2:514], in_=psb(b), func=AF.Identity,
                                                                  bias=C("bup", jj, jj + 1)),
                    R=[R_ps[b], R_cst], W=[R_stg[sg]])
              kb.op(DVE, lambda jj=jj, sg=sg: ve.tensor_copy(out=halo_f[:, jj, 0:2], in_=stg[:, sg, 512:514]),
                    R=[R_stg[sg]], W=[R_hf[jj]])
              return sl, rg, b, sg, jj

          def ffn_b(it, ctx):
              j, part = it
              sl, rg, b, sg, jj = ctx
              b2 = 2 + b
              for k in range(3):
                  kb.op(PE, lambda k=k, b2=b2, sl=sl, sg=sg: te.matmul(
                      psb(b2), sl[:, 1024 + k * 128:1024 + (k + 1) * 128], stg[:, sg, k:k + 512],
                      start=(k == 0), stop=(k == 2)), R=[rg, R_stg[sg]], W=[R_ps[b2]])
              if part == 0:
                  kb.op(ACT, lambda b2=b2, jj=jj: se.activation(out=asil[:, :], in_=psb(b2), func=AF.Silu,
                                                                bias=C("fcb", jj, jj + 1)),
                        R=[R_ps[b2], R_cst], W=[R_asil])
              else:
                  kb.op(DVE, lambda b2=b2, jj=jj, j=j: ve.scalar_tensor_tensor(
                      out=gT2[:, j, :], in0=psb(b2), scalar=C("fcb", jj, jj + 1), in1=asil[:, :],
                      op0=ALU.add, op1=ALU.mult), R=[R_ps[b2], R_cst, R_asil], W=[R_g2[j]])

          pipelined([(j, part) for j in range(22) for part in (0, 1)], ffn_a, ffn_b)
          for half in range(2):
              for q in range(6):
                  sl, rg = unit("down", (half, q))
                  nk = 4 if q < 5 else 2
                  for c in range(4):
                      for k4 in range(nk):
                          kc = q * 4 + k4
                          kb.op(PE, lambda c=c, kc=kc, k4=k4, sl=sl: te.matmul(
                              psb(4 + c), gT2[:, kc, c * 128:(c + 1) * 128], sl[:, k4 * 512:(k4 + 1) * 512],
                              start=(kc == 0), stop=(kc == 21)), R=[rg, R_g2[kc]], W=[R_ps[4 + c]])
              for c in range(4):
                  kb.op(DVE, lambda c=c: ve.scalar_tensor_tensor(
                      out=XB[:, c, half * 512:(half + 1) * 512], in0=XB[:, c, half * 512:(half + 1) * 512],
                      scalar=ALPHA, in1=psb(4 + c), op0=ALU.mult, op1=ALU.add),
                      R=[R_XB[c], R_ps[4 + c]], W=[R_XB[c]])
          ln3 = lambda: layer_norm_all("ln3g", "ln3b", False, after=lambda c: kb.dma(
              POOL, f"xb{c}", out_d[t0 + c * 128:t0 + (c + 1) * 128, :], XB[:, c, :], R=[R_XB[c]]))
          if t + 1 < NT:
              prologue(t + 1)
              la = record(ln3)
              lb = record(lambda: pipelined(range(12), xbc_a, xbc_b))
              for th in merge(la, lb):
                  th()
          else:
              ln3()

    except _Stop:
        pass
    kb.wait_all_dma(SP, list(kb.dsem.keys()))
    kb.es.close()
    return nc


def _core_inputs(inp, b, shared):
    ws, wo, cst, cs = shared
    x = np.asarray(inp["x"], np.float32)[b]
    mem = np.asarray(inp["mem"], np.float32)[b]
    return {
        "xT": np.ascontiguousarray(x.T.reshape(8, 128, L).transpose(1, 0, 2)),
        "xn": np.ascontiguousarray(x),
        "memT": np.ascontiguousarray(mem.T.reshape(8, 128, MEM).transpose(1, 0, 2)),
        "ws": ws, "wo": wo, "cst": cst, "cs": cs,
    }


def kernel(**inputs):
    shared = _host_prep(inputs)
    nc = _build()
    in_maps = [_core_inputs(inputs, b, shared) for b in range(8)]
    res = run_bass_kernel_spmd(nc, in_maps, core_ids=list(range(8)))
    return np.stack([np.asarray(r["out"], np.float32) for r in res.results], axis=0)
```

```python
import numpy as np
from contextlib import ExitStack
import concourse.bass as bass
import concourse.mybir as mybir
from concourse.bass_utils import run_bass_kernel_spmd

F32 = mybir.dt.float32
BF16 = mybir.dt.bfloat16
AF = mybir.ActivationFunctionType
ALU = mybir.AluOpType
AX = mybir.AxisListType

D = 1024
L = 8192
T = 512
NTILES = L // T
MEM = 256
DFF = 2752
ALPHA = 2.0 ** 0.25
EPS = 1e-5
OFF_Z, OFF_XBC, OFF_DT, OFF_Q, OFF_K, OFF_V, OFF_G = 0, 1024, 2560, 2576, 3088, 3600, 4624
NSLOT = 5
SLOT = 2048
EPOCH = 16000

_CST = {}
_off = 0
for _n, _w in [("ident", 128), ("tri", 128), ("strict", 128), ("ones", 128), ("prot", 128),
               ("decT", 1024), ("qdecT", 512), ("kdec", 8), ("cdr", 4),
               ("ln1g", 1024), ("ln1b", 1024), ("ln2g", 1024), ("ln2b", 1024), ("ln3g", 1024), ("ln3b", 1024),
               ("dtb", 16), ("alog", 16), ("drow", 16), ("scb", 12), ("normw", 8), ("gnw", 8), ("gnb", 8),
               ("bup", 44), ("fcb", 44)]:
    _CST[_n] = (_off, _w)
    _off += _w
NCST = _off


def _stream_units():
    u = []
    for j in range(12):
        u.append(("xbc", j, 1024 + 512))
    for j in range(8):
        u.append(("qk", j, 1024))
    for j in range(8):
        u.append(("g", j, 1024))
    for blk in range(2):
        for hk in range(2):
            u.append(("z", (blk, hk), 2048))
    for blk in range(2):
        for hk in range(2):
            u.append(("v", (blk, hk), 2048))
    u.append(("dt", 0, 128))
    for half in range(2):
        for q in range(4):
            u.append(("wout", (half, q), 2048))
    for j in range(8):
        u.append(("xq", j, 1024))
    for half in range(2):
        for q in range(2):
            u.append(("xo", (half, q), 2048))
    for j in range(22):
        u.append(("fa", j, 1024 + 384))
        u.append(("fu", j, 1024 + 384))
    for half in range(2):
        for q in range(6):
            u.append(("down", (half, q), 2048))
    return u


UNITS = _stream_units()
UOFF = np.cumsum([0] + [x[2] for x in UNITS]).tolist()
WLEN = UOFF[-1]
ONCE_UNITS = [("xk", j, 1024) for j in range(8)] + [("xv", (b, h), 2048) for b in range(2) for h in range(2)]
OOFF = np.cumsum([0] + [x[2] for x in ONCE_UNITS]).tolist()
WOLEN = OOFF[-1]


def _fm(w, col0, ncols=128):
    K = w.shape[0]
    blk = np.zeros((K, 128), np.float32)
    c1 = min(col0 + ncols, w.shape[1])
    blk[:, : c1 - col0] = w[:, col0:c1]
    return blk.reshape(K // 128, 128, 128).transpose(1, 0, 2).reshape(128, -1)


def _tm(w, kc0, nk, col0):
    out = np.zeros((128, nk, 512), np.float32)
    for kc in range(nk):
        r0 = (kc0 + kc) * 128
        r1 = min(r0 + 128, w.shape[0])
        if r1 > r0:
            out[: r1 - r0, kc, :] = w[r0:r1, col0:col0 + 512]
    return out.reshape(128, -1)


def _diag(cw, col0):
    nt = cw.shape[0]
    out = np.zeros((128, nt, 128), np.float32)
    n = min(128, cw.shape[1] - col0)
    idx = np.arange(n)
    for k in range(nt):
        out[idx, k, idx] = cw[k, col0:col0 + n]
    return out.reshape(128, -1)


def _pp(v, n):
    buf = np.zeros(n * 128, np.float32)
    buf[: v.shape[0]] = v
    return buf.reshape(n, 128).T


def _host_prep(inp):
    f = lambda k: np.asarray(inp[k], np.float32)[0]
    w_in, w_out = f("w_in"), f("w_mix_out")
    ws = np.zeros((128, WLEN), np.float32)
    for i, (kind, key, n) in enumerate(UNITS):
        o = UOFF[i]
        if kind == "xbc":
            ws[:, o:o + 1024] = _fm(w_in, OFF_XBC + key * 128)
            ws[:, o + 1024:o + 1536] = _diag(f("ssd_conv_w"), key * 128)
        elif kind == "qk":
            ws[:, o:o + n] = _fm(w_in, OFF_Q + key * 128)
        elif kind == "g":
            ws[:, o:o + n] = _fm(w_in, OFF_G + key * 128)
        elif kind == "z":
            ws[:, o:o + n] = _tm(w_in, key[1] * 4, 4, OFF_Z + key[0] * 512)
        elif kind == "v":
            ws[:, o:o + n] = _tm(w_in, key[1] * 4, 4, OFF_V + key[0] * 512)
        elif kind == "dt":
            ws[:, o:o + n] = w_in[:, OFF_DT:OFF_DT + 16].reshape(8, 128, 16).transpose(1, 0, 2).reshape(128, -1)
        elif kind == "wout":
            ws[:, o:o + n] = _tm(w_out, key[1] * 4, 4, key[0] * 512)
        elif kind == "xq":
            ws[:, o:o + n] = _fm(f("w_xq"), key * 128)
        elif kind == "xo":
            ws[:, o:o + n] = _tm(f("w_xo"), key[1] * 4, 4, key[0] * 512)
        elif kind == "fa":
            ws[:, o:o + 1024] = _fm(f("w_ffn_up")[:, :DFF], key * 128)
            ws[:, o + 1024:o + 1408] = _diag(f("ffn_conv_w")[:, :DFF], key * 128)
        elif kind == "fu":
            ws[:, o:o + 1024] = _fm(f("w_ffn_up")[:, DFF:], key * 128)
            ws[:, o + 1024:o + 1408] = _diag(f("ffn_conv_w")[:, DFF:], key * 128)
        elif kind == "down":
            ws[:, o:o + n] = _tm(f("w_ffn_down"), key[1] * 4, 4, key[0] * 512)
    wo = np.zeros((128, WOLEN), np.float32)
    for i, (kind, key, n) in enumerate(ONCE_UNITS):
        o = OOFF[i]
        if kind == "xk":
            wo[:, o:o + n] = _fm(f("w_xk"), key * 128)
        else:
            wo[:, o:o + n] = _tm(f("w_xv"), key[1] * 4, 4, key[0] * 512)

    cst = np.zeros((128, NCST), np.float32)

    def put(name, arr):
        o, w = _CST[name]
        cst[:, o:o + w] = np.asarray(arr, np.float32).reshape(128, w)

    ar = np.arange(128)
    put("ident", np.eye(128))
    put("tri", (ar[:, None] <= ar[None, :]))
    put("strict", (ar[:, None] > ar[None, :]))
    put("ones", np.ones((128, 128)))
    prot = np.zeros((128, 128))
    for m in range(128):
        if (m % 64) < 32:
            prot[m + 32, m] = -1.0
        else:
            prot[m - 32, m] = 1.0
    put("prot", prot)
    gam = 1.0 - np.exp2(-5.0 - np.arange(8, dtype=np.float64))
    decT = np.zeros((128, 8, 128))
    for h in range(8):
        dist = ar[None, :] - ar[:, None]
        decT[:, h, :] = np.where(dist >= 0, 0.125 * gam[h] ** np.maximum(dist, 0), 0.0)
    put("decT", decT)
    qd = np.zeros((128, 4, 128))
    cdr = np.zeros((128, 4))
    for p in range(128):
        for j in range(4):
            h = 2 * j + p // 64
            qd[p, j, :] = gam[h] ** (ar + 1.0)
            cdr[p, j] = gam[h] ** 128.0
    put("qdecT", qd)
    put("cdr", cdr)
    put("kdec", 0.125 * gam[None, :] ** (127.0 - ar[:, None]))
    for nm, key in (("ln1g", "ln1_g"), ("ln1b", "ln1_b"), ("ln2g", "ln2_g"), ("ln2b", "ln2_b"),
                    ("ln3g", "ln3_g"), ("ln3b", "ln3_b")):
        put(nm, np.broadcast_to(f(key)[None, :], (128, 1024)))
    put("dtb", np.broadcast_to(f("ssd_dt_bias")[None, :], (128, 16)))
    put("alog", np.broadcast_to(f("ssd_a_log")[None, :], (128, 16)))
    put("drow", np.broadcast_to(f("ssd_d")[None, :], (128, 16)))
    put("scb", _pp(f("ssd_conv_b"), 12))
    put("normw", _pp(f("ssd_norm_w"), 8))
    put("gnw", _pp(f("ret_gn_w"), 8))
    put("gnb", _pp(f("ret_gn_b"), 8))
    bup, fcb = f("b_ffn_up"), f("ffn_conv_b")
    put("bup", np.concatenate([_pp(bup[:DFF], 22), _pp(bup[DFF:], 22)], axis=1))
    put("fcb", np.concatenate([_pp(fcb[:DFF], 22), _pp(fcb[DFF:], 22)], axis=1))

    pos = np.arange(L, dtype=np.float32)
    freqs = (1.0 / (np.float32(10000.0) ** np.linspace(0.0, 1.0, 32, dtype=np.float32))).astype(np.float32)
    ang = (pos[:, None] * freqs[None, :]).astype(np.float32).astype(np.float64)
    angp = ang[:, np.arange(128) % 32].T
    cs = np.zeros((NTILES, 128, 1024), np.float32)
    for t in range(NTILES):
        cs[t, :, :512] = np.cos(angp[:, t * T:(t + 1) * T])
        cs[t, :, 512:] = np.sin(angp[:, t * T:(t + 1) * T])
    return ws, wo, cst, cs


class _Reg:
    __slots__ = ("name", "w", "r", "excl")

    def __init__(self, name, excl=False):
        self.name = name
        self.w = None
        self.r = {}
        self.excl = excl


class _Eng:
    def __init__(self, kb, name, h, self_raw):
        self.kb, self.name, self.h, self.self_raw = kb, name, h, self_raw
        self.n = 0
        self.sems = []
        self.seen = {}

    def cur(self):
        e = self.n // EPOCH
        while len(self.sems) <= e:
            self.sems.append(self.kb.es.enter_context(self.kb.nc.semaphore(f"s_{self.name}_{len(self.sems)}")))
        return (self.name, e), self.sems[e], self.n % EPOCH + 1


class _KB:
    def __init__(self):
        self.nc = bass.Bass("TRN2", target_bir_lowering=False)
        self.es = ExitStack()
        nc = self.nc
        self.pe = _Eng(self, "pe", nc.tensor, False)
        self.act = _Eng(self, "act", nc.scalar, True)
        self.dve = _Eng(self, "dve", nc.vector, True)
        self.pool = _Eng(self, "pool", nc.gpsimd, True)
        self.sp = _Eng(self, "sp", nc.sync, False)
        self.dsem = {}
        self.dval = {}

    def sb(self, name, shape, dt):
        return self.es.enter_context(self.nc.sbuf_tensor("sb_" + name, shape, dt))

    def _waits(self, E, R, W):
        deps = {}

        def add(tok, kind):
            if tok is None:
                return
            key, sem, val, src = tok
            if src is E and (kind == "rr" or not E.self_raw):
                return
            if key not in deps or deps[key][1] < val:
                deps[key] = (sem, val)

        for r in R:
            add(r.w, "raw")
            if r.excl:
                for t in r.r.values():
                    add(t, "rr")
        for w in W:
            add(w.w, "waw")
            for t in w.r.values():
                add(t, "war")
        for key, (sem, val) in deps.items():
            if E.seen.get(key, 0) < val:
                E.h.wait_ge(sem, val)
                E.seen[key] = val

    def _mark(self, tok, R, W):
        key = tok[0]
        for r in R:
            old = r.r.get(key)
            if old is None or old[2] < tok[2]:
                r.r[key] = tok
        for w in W:
            w.w = tok
            w.r = {}

    def op(self, E, fn, R=(), W=()):
        self._waits(E, R, W)
        inst = fn()
        key, sem, val = E.cur()
        inst.then_inc(sem, 1)
        E.n += 1
        self._mark((key, sem, val, E), R, W)

    def dma(self, Q, semname, out, in_, R=(), W=()):
        self._waits(Q, R, W)
        if semname not in self.dsem:
            self.dsem[semname] = self.es.enter_context(self.nc.semaphore("d_" + semname))
            self.dval[semname] = 0
        d = Q.h.dma_start(out=out, in_=in_)
        self.dval[semname] += 16
        d.then_inc(self.dsem[semname], 16)
        self._mark((("dma", semname), self.dsem[semname], self.dval[semname], None), R, W)

    def wait_all_dma(self, Q, names):
        for n in names:
            Q.h.wait_ge(self.dsem[n], self.dval[n])


class _Stop(Exception):
    pass


def _build(NT=NTILES, dbg=False, stop=None):
    kb = _KB()
    nc = kb.nc
    PE, ACT, DVE, POOL, SP = kb.pe, kb.act, kb.dve, kb.pool, kb.sp
    te, se, ve, ge = nc.tensor, nc.scalar, nc.vector, nc.gpsimd

    xT_d = nc.dram_tensor("xT", [128, 8, L], F32, kind="ExternalInput").ap()
    xn_d = nc.dram_tensor("xn", [L, D], F32, kind="ExternalInput").ap()
    memT_d = nc.dram_tensor("memT", [128, 8, MEM], F32, kind="ExternalInput").ap()
    ws_d = nc.dram_tensor("ws", [128, WLEN], F32, kind="ExternalInput").ap()
    wo_d = nc.dram_tensor("wo", [128, WOLEN], F32, kind="ExternalInput").ap()
    cst_d = nc.dram_tensor("cst", [128, NCST], F32, kind="ExternalInput").ap()
    cs_d = nc.dram_tensor("cs", [NTILES, 128, 1024], F32, kind="ExternalInput").ap()
    out_d = nc.dram_tensor("out", [L, D], F32, kind="ExternalOutput").ap()
    wsb_d = nc.dram_tensor("wsb", [128, WLEN], BF16, kind="Internal").ap()
    R_wsb = [_Reg(f"wsb{i}") for i in range(len(UNITS))]
    dbg_d = {}
    if dbg:
        dbg_d["ymix"] = nc.dram_tensor("dbg_ymix", [128, 16 * 512], BF16, kind="ExternalOutput").ap()
        dbg_d["x1"] = nc.dram_tensor("dbg_x1", [128, 4 * 1024], F32, kind="ExternalOutput").ap()
        dbg_d["x2"] = nc.dram_tensor("dbg_x2", [128, 4 * 1024], F32, kind="ExternalOutput").ap()

    sb = kb.sb
    cst = sb("cst", [128, NCST], F32)
    R_cst = _Reg("cst")

    def C(name, a=None, b=None):
        o, w = _CST[name]
        if a is None:
            return cst[:, o:o + w]
        return cst[:, o + a:o + b]

    ident_bf = sb("ident_bf", [128, 128], BF16)
    prot_bf = sb("prot_bf", [128, 128], BF16)
    arow = sb("arow", [128, 16], F32)
    R_c2 = _Reg("c2")
    ring = sb("ring", [128, NSLOT, SLOT], BF16)
    R_ring = [_Reg(f"ring{i}") for i in range(NSLOT)]
    xTb = sb("xTb", [128, 8, T], BF16)
    R_xT = [_Reg(f"xT{c}") for c in range(4)]
    P0 = sb("P0", [128, 24576], BF16)
    xbcT = P0[:, 0:6144].rearrange("p (j t) -> p j t", j=12)
    qkT = P0[:, 6144:10240].rearrange("p (j t) -> p j t", j=8)
    qsT = P0[:, 10240:12288].rearrange("p (j t) -> p j t", j=4)
    zs = P0[:, 12288:16384].rearrange("p (c n) -> p c n", c=4)
    vt = P0[:, 16384:20480].rearrange("p (c n) -> p c n", c=4)
    gT = P0[:, 20480:24576].rearrange("p (j t) -> p j t", j=8)
    R_xbc = [_Reg(f"xbc{j}") for j in range(12)]
    R_qk = [_Reg(f"qk{j}") for j in range(8)]
    R_qs = [_Reg(f"qs{j}") for j in range(4)]
    R_zs = [_Reg(f"zs{c}") for c in range(4)]
    R_v = [_Reg(f"v{c}") for c in range(4)]
    R_g = [_Reg(f"g{j}") for j in range(8)]
    qxT = P0[:, 0:4096].rearrange("p (j t) -> p j t", j=8)
    oT = P0[:, 4096:8192].rearrange("p (j t) -> p j t", j=8)
    gT2 = P0[:, 8192:8192 + 22 * 512].rearrange("p (j t) -> p j t", j=22)
    R_qx = [_Reg(f"qx{j}") for j in range(8)]
    R_oT = [_Reg(f"oT{c}") for c in range(4)]
    R_g2 = [_Reg(f"g2{j}") for j in range(22)]
    view_a = R_xbc + R_qk + R_qs + R_zs + R_v + R_g
    view_b = R_qx + R_oT + R_g2
    fence = sb("fence", [128, 2], F32)
    R_fence = _Reg("fence")

    ymixT = sb("ymixT", [128, 16, T], BF16)
    R_ym = [_Reg(f"ym{c}") for c in range(4)]
    XB = sb("XB", [128, 4, D], F32)
    R_XB = [_Reg(f"XB{c}") for c in range(4)]
    hT = sb("hT", [128, 1024], F32)
    hT_bf = sb("hT_bf", [128, 1024], BF16)
    rS = sb("rS", [128, 512], F32)
    rS_bf = sb("rS_bf", [128, 512], BF16)
    R_hT, R_hTb, R_rS, R_rSb = _Reg("hT"), _Reg("hTb"), _Reg("rS"), _Reg("rSb")
    halo_s = sb("halo_s", [128, 12, 4], BF16)
    halo_f = sb("halo_f", [128, 44, 2], BF16)
    R_hs = [_Reg(f"hs{j}") for j in range(12)]
    R_hf = [_Reg(f"hf{j}") for j in range(44)]
    kmT = sb("kmT", [128, 8, MEM], BF16)
    vm = sb("vm", [128, 2, D], BF16)
    R_km, R_vm = _Reg("km"), _Reg("vm")
    stg = sb("stg", [128, 2, 516], BF16)
    R_stg = [_Reg("stg0"), _Reg("stg1")]
    raw = sb("raw", [128, 2, 512], BF16)
    R_raw = [_Reg("raw0"), _Reg("raw1")]
    rt1 = sb("rt1", [128, 512], F32)
    rt2 = sb("rt2", [128, 512], F32)
    R_rt1, R_rt2 = _Reg("rt1"), _Reg("rt2")
    dt_all = sb("dt_all", [128, 4, 16], F32)
    dt_tmp = sb("dt_tmp", [128, 4, 16], F32)
    R_dt, R_dtt = _Reg("dt"), _Reg("dtt")
    a_all = sb("a_all", [128, 4, 16], F32)
    R_a = _Reg("a")
    rhs_a = sb("rhs_a", [128, 16, 128], F32)
    R_rhsa = [_Reg("rhsa0"), _Reg("rhsa1")]
    segT = sb("segT", [128, 16, 128], BF16)
    R_seg = [_Reg("seg0"), _Reg("seg1")]
    attT = sb("attT", [128, 16, 128], BF16)
    R_att = [_Reg("att0"), _Reg("att1")]
    cbm = sb("cbm", [128, 2, 128], BF16)
    R_cbm = _Reg("cbm")
    xdt = sb("xdt", [128, 1024], BF16)
    xdtd = sb("xdtd", [128, 1024], BF16)
    R_xdt, R_xdtd = _Reg("xdt"), _Reg("xdtd")
    dtd = sb("dtd", [128, 16], F32)
    R_dtd = _Reg("dtd")
    F1 = sb("F1", [128, 1024], F32)
    F2 = sb("F2", [128, 1024], F32)
    R_F1, R_F2 = _Reg("F1"), _Reg("F2")
    cs_sb, R_cs = F2, R_F2
    H1 = sb("H1", [128, 1024], BF16)
    H2 = sb("H2", [128, 1024], BF16)
    R_H1, R_H2 = _Reg("H1"), _Reg("H2")
    Btok = sb("Btok", [128, 2, 128], BF16)
    R_Btok = _Reg("Btok")
    kd = sb("kd", [128, 512], BF16)
    R_kd = _Reg("kd")
    sT = sb("sT", [128, 8, 128], BF16)
    R_sT = _Reg("sT")
    pT = sb("pT", [128, 8, 128], BF16)
    R_pT = _Reg("pT")
    asil = sb("asil", [128, 512], BF16)
    R_asil = _Reg("asil")
    sm = sb("sm", [128, 64], F32)
    R_sm = _Reg("sm")
    R_smS, R_smR = _Reg("smS"), _Reg("smR")
    dfcd = sb("dfcd", [128, 32], F32)
    R_dfcd = _Reg("dfcd")
    bnst = sb("bnst", [128, 2, 6], F32)
    bnmv = sb("bnmv", [128, 2], F32)
    R_bn = _Reg("bn")

    ps = kb.es.enter_context(nc.psum_tensor("ps", [128, 8, 512], F32))
    R_ps = [_Reg(f"ps{i}", excl=True) for i in range(8)]

    def psb(b, a=0, e=512):
        return ps[:, b, a:e]

    def ps2(b):
        return ps[:, b:b + 2, :].rearrange("p a b -> p (a b)")

    def psbf(b):
        return ps[:, b, :].bitcast(BF16)

    state = {"next": 0, "use": 0, "once_next": 0}
    total_units = len(UNITS) * NT

    def issue_until(g):
        while state["next"] <= min(g, total_units - 1):
            n = state["next"]
            i = n % len(UNITS)
            s = n % NSLOT
            ncols = UNITS[i][2]
            if n < len(UNITS):
                kb.dma(POOL, f"ring{s}", ring[:, s, 0:ncols], ws_d[:, UOFF[i]:UOFF[i] + ncols], W=[R_ring[s]])
                if NT > 1:
                    kb.dma(SP, f"wb{s}", wsb_d[:, UOFF[i]:UOFF[i] + ncols], ring[:, s, 0:ncols],
                           R=[R_ring[s]], W=[R_wsb[i]])
            else:
                kb.dma(SP, f"ringh{s}", ring[:, s, 0:ncols], wsb_d[:, UOFF[i]:UOFF[i] + ncols],
                       R=[R_wsb[i]], W=[R_ring[s]])
            state["next"] += 1

    def unit(kind, key, hold=0):
        g = state["use"]
        i = g % len(UNITS)
        assert UNITS[i][0] == kind and UNITS[i][1] == key, (UNITS[i], kind, key)
        issue_until(g + NSLOT - 1 - hold)
        state["use"] += 1
        s = g % NSLOT
        return ring[:, s, :], R_ring[s]

    kb.dma(POOL, "cst", cst[:, :], cst_d[:, :], W=[R_cst])
    kb.op(DVE, lambda: ve.tensor_copy(out=ident_bf[:, :], in_=C("ident")), R=[R_cst], W=[R_c2])
    kb.op(DVE, lambda: ve.tensor_copy(out=prot_bf[:, :], in_=C("prot")), R=[R_cst], W=[R_c2])
    kb.op(ACT, lambda: se.activation(out=arow[:, :], in_=C("alog"), func=AF.Exp), R=[R_cst], W=[R_c2])
    kb.op(DVE, lambda: ve.tensor_scalar(out=arow[:, :], in0=arow[:, :], scalar1=-1.0, scalar2=None, op0=ALU.mult),
          R=[R_c2], W=[R_c2])
    kb.op(DVE, lambda: ve.memset(hT[:, :], 0.0), W=[R_hT])
    kb.op(DVE, lambda: ve.memset(hT_bf[:, :], 0.0), W=[R_hTb])
    kb.op(DVE, lambda: ve.memset(rS[:, :], 0.0), W=[R_rS])
    kb.op(DVE, lambda: ve.memset(rS_bf[:, :], 0.0), W=[R_rSb])
    kb.op(DVE, lambda: ve.memset(halo_s[:, :, :], 0.0), W=R_hs)
    kb.op(DVE, lambda: ve.memset(halo_f[:, :, :], 0.0), W=R_hf)
    kb.op(DVE, lambda: ve.memset(fence[:, :], 0.0), W=[R_fence])

    memTb = xTb[:, :, 0:MEM]
    kb.dma(POOL, "xT", memTb, memT_d[:, :, :], W=R_xT)
    pa_i = [0]

    def next_pa():
        b = pa_i[0] % 2
        pa_i[0] += 1
        return b

    for i, (kind, key, n) in enumerate(ONCE_UNITS):
        s = i % NSLOT
        kb.dma(POOL, f"ring{s}", ring[:, s, 0:n], wo_d[:, OOFF[i]:OOFF[i] + n], W=[R_ring[s]])
        if kind == "xk":
            b = next_pa()
            for kc in range(8):
                kb.op(PE, lambda kc=kc, s=s, b=b: te.matmul(psb(b, 0, MEM), ring[:, s, kc * 128:(kc + 1) * 128],
                                                           memTb[:, kc, :], start=(kc == 0), stop=(kc == 7)),
                      R=[R_ring[s]] + R_xT, W=[R_ps[b]])
            kb.op(ACT, lambda b=b, key=key: se.activation(out=kmT[:, key, :], in_=psb(b, 0, MEM), func=AF.Copy),
                  R=[R_ps[b]], W=[R_km])
        else:
            blk, hk = key
            if hk == 1:
                s0 = (i - 1) % NSLOT
                for mc in range(2):
                    b = next_pa()
                    for kc in range(8):
                        ss = s0 if kc < 4 else s
                        kb.op(PE, lambda kc=kc, ss=ss, b=b, mc=mc: te.matmul(
                            psb(b), memTb[:, kc, mc * 128:(mc + 1) * 128],
                            ring[:, ss, (kc % 4) * 512:(kc % 4 + 1) * 512], start=(kc == 0), stop=(kc == 7)),
                            R=[R_ring[s0], R_ring[s]] + R_xT, W=[R_ps[b]])
                    kb.op(ACT, lambda b=b, mc=mc, blk=blk: se.activation(
                        out=vm[:, mc, blk * 512:(blk + 1) * 512], in_=psb(b), func=AF.Copy),
                        R=[R_ps[b]], W=[R_vm])

    bnst4 = sb("bnst4", [128, 4, 12], F32)
    bnmv4 = sb("bnmv4", [128, 4, 2], F32)
    R_bn4 = [_Reg(f"bn4_{c}") for c in range(4)]
    R_sm4 = [_Reg(f"sm4_{c}") for c in range(4)]

    def layer_norm_all(gname, bname, make_T, after=None):
        for c in range(4):
            for hlf in range(2):
                kb.op(DVE, lambda c=c, hlf=hlf: ve.bn_stats(out=bnst4[:, c, hlf * 6:(hlf + 1) * 6],
                                                            in_=XB[:, c, hlf * 512:(hlf + 1) * 512]),
                      R=[R_XB[c]], W=[R_bn4[c]])
            kb.op(DVE, lambda c=c: ve.bn_aggr(out=bnmv4[:, c, :], in_=bnst4[:, c, :]), R=[R_bn4[c]], W=[R_bn4[c]])
            kb.op(DVE, lambda c=c: ve.tensor_scalar(out=sm[:, 48 + 2 * c:49 + 2 * c], in0=bnmv4[:, c, 1:2], scalar1=EPS,
                                                    scalar2=None, op0=ALU.add), R=[R_bn4[c]], W=[R_sm4[c]])
        for c in range(4):
            kb.op(ACT, lambda c=c: se.activation(out=sm[:, 48 + 2 * c:49 + 2 * c], in_=sm[:, 48 + 2 * c:49 + 2 * c],
                                                 func=AF.Ln), R=[R_sm4[c]], W=[R_sm4[c]])
            kb.op(ACT, lambda c=c: se.activation(out=sm[:, 48 + 2 * c:49 + 2 * c], in_=sm[:, 48 + 2 * c:49 + 2 * c],
                                                 func=AF.Exp, scale=-0.5), R=[R_sm4[c]], W=[R_sm4[c]])
        for c in range(4):
            kb.op(DVE, lambda c=c: ve.scalar_tensor_tensor(out=sm[:, 49 + 2 * c:50 + 2 * c], in0=bnmv4[:, c, 0:1], scalar=-1.0,
                                                           in1=sm[:, 48 + 2 * c:49 + 2 * c], op0=ALU.mult, op1=ALU.mult),
                  R=[R_sm4[c], R_bn4[c]], W=[R_sm4[c]])
        for c in range(4):
            kb.op(ACT, lambda c=c: se.activation(out=XB[:, c, :], in_=XB[:, c, :], func=AF.Identity,
                                                 bias=sm[:, 49 + 2 * c:50 + 2 * c], scale=sm[:, 48 + 2 * c:49 + 2 * c]),
                  R=[R_XB[c], R_sm4[c]], W=[R_XB[c]])
        for c in range(4):
            kb.op(DVE, lambda c=c: ve.tensor_tensor(out=XB[:, c, :], in0=XB[:, c, :], in1=C(gname), op=ALU.mult),
                  R=[R_XB[c], R_cst], W=[R_XB[c]])
            kb.op(DVE, lambda c=c: ve.tensor_tensor(out=XB[:, c, :], in0=XB[:, c, :], in1=C(bname), op=ALU.add),
                  R=[R_XB[c], R_cst], W=[R_XB[c]])
            if after is not None:
                after(c)
        if make_T:
            for c in range(4):
                Hc, RHc, bk = ((H1, R_H1, 5), (H2, R_H2, 6))[c % 2]
                kb.op(ACT, lambda c=c, Hc=Hc: se.activation(out=Hc[:, :], in_=XB[:, c, :], func=AF.Copy),
                      R=[R_XB[c]], W=[RHc])
                for j in range(8):
                    kb.op(PE, lambda j=j, Hc=Hc, bk=bk: te.transpose(out=psbf(bk)[:, j * 128:(j + 1) * 128],
                                                                     in_=Hc[:, j * 128:(j + 1) * 128],
                                                                     identity=ident_bf[:, :]),
                          R=[RHc, R_c2], W=[R_ps[bk]])
                kb.op(ACT, lambda c=c, bk=bk: se.activation(out=xTb[:, :, c * 128:(c + 1) * 128],
                                                           in_=psbf(bk).rearrange("p (j t) -> p j t", j=8), func=AF.Copy),
                      R=[R_ps[bk]], W=[R_xT[c]])

    def proj_tm_accum(kind, nq, lhs_of, lhs_regs, half):
        for q in range(nq):
            sl, rg = unit(kind, (half, q))
            for c in range(4):
                for k4 in range(4):
                    kc = q * 4 + k4
                    kb.op(PE, lambda c=c, kc=kc, k4=k4, sl=sl: te.matmul(
                        psb(c), lhs_of(kc, c), sl[:, k4 * 512:(k4 + 1) * 512],
                        start=(kc == 0), stop=(kc == nq * 4 - 1)),
                        R=[rg] + lhs_regs(kc, c), W=[R_ps[c]])

    def residual_from_psum(half):
        for c in range(4):
            kb.op(DVE, lambda c=c: ve.scalar_tensor_tensor(
                out=XB[:, c, half * 512:(half + 1) * 512], in0=XB[:, c, half * 512:(half + 1) * 512],
                scalar=ALPHA, in1=psb(c), op0=ALU.mult, op1=ALU.add),
                R=[R_XB[c], R_ps[c]], W=[R_XB[c]])

    def chk(name):
        if stop == name:
            raise _Stop()

    DEF_COST = {"pe": 0.12, "act": 0.55, "dve": 0.9, "pool": 2.0, "sp": 0.0}

    def record(body):
        rec = []
        real_op, real_dma = kb.op, kb.dma
        kb.op = lambda E, fn, R=(), W=(): rec.append(
            (lambda: real_op(E, fn, list(R), list(W)), E.name, list(R), list(W), DEF_COST[E.name]))
        kb.dma = lambda Q, sn, out, in_, R=(), W=(): rec.append(
            (lambda: real_dma(Q, sn, out, in_, list(R), list(W)), Q.name, list(R), list(W), 0.0))
        try:
            body()
        finally:
            del kb.op
            del kb.dma
        return rec

    def merge(la, lb):
        out, i, j = [], 0, 0
        while i < len(la) or j < len(lb):
            if j >= len(lb) or (i < len(la) and i * len(lb) <= j * len(la)):
                out.append(la[i]); i += 1
            else:
                out.append(lb[j]); j += 1
        return out

    def merge_sched(streams, hop=1.6):
        eng_free, wdone, weng, rdone = {}, {}, {}, {}
        ptr = [0] * len(streams)
        out = []

        def start_time(it):
            _, en, R, W, _c = it
            t = eng_free.get(en, 0.0)
            for r in R:
                if id(r) in wdone:
                    t = max(t, wdone[id(r)] + (hop if weng[id(r)] != en else 0.1))
            for w in W:
                if id(w) in wdone:
                    t = max(t, wdone[id(w)] + (hop if weng[id(w)] != en else 0.0))
                if id(w) in rdone:
                    t = max(t, rdone[id(w)] + hop)
            return t

        while True:
            best, bt = None, None
            for k, st in enumerate(streams):
                if ptr[k] < len(st):
                    t = start_time(st[ptr[k]])
                    if bt is None or t < bt - 1e-9:
                        best, bt = k, t
            if best is None:
                break
            it = streams[best][ptr[best]]
            ptr[best] += 1
            _, en, R, W, c = it
            end = bt + c
            eng_free[en] = end
            for w in W:
                wdone[id(w)] = end
                weng[id(w)] = en
                rdone.pop(id(w), None)
            for r in R:
                rdone[id(r)] = max(rdone.get(id(r), 0.0), end)
            out.append(it)
        return out

    def pipelined(items, phase_a, phase_b):
        prev = None
        for it in items:
            ctx = phase_a(it)
            if prev is not None:
                phase_b(*prev)
            prev = (it, ctx)
        phase_b(*prev)

    try:
      chk("s0")
      for t in range(NT):
          t0 = t * T
          def prologue(tt):
              kb.op(DVE, lambda: ve.memset(fence[:, 0:1], 0.0), R=view_b, W=view_a + [R_fence])
              kb.dma(POOL, "xT", xTb[:, :, :], xT_d[:, :, tt * T:(tt + 1) * T], W=R_xT)
              kb.dma(POOL, "cs", cs_sb[:, :], cs_d[tt, :, :], W=[R_cs])

          if t == 0:
              prologue(0)
          for c in range(4):
              kb.dma(POOL, f"xb{c}", XB[:, c, :], xn_d[t0 + c * 128:t0 + (c + 1) * 128, :], W=[R_XB[c]])

          def xbc_a(j):
              sl, rg = unit("xbc", j, hold=1)
              b = next_pa()
              sg = j % 2
              for kc in range(8):
                  kb.op(PE, lambda kc=kc, b=b, sl=sl: te.matmul(psb(b), sl[:, kc * 128:(kc + 1) * 128], xTb[:, kc, :],
                                                                start=(kc == 0), stop=(kc == 7)),
                        R=[rg] + R_xT, W=[R_ps[b]])
              kb.op(DVE, lambda j=j, sg=sg: ve.tensor_copy(out=stg[:, sg, 0:3], in_=halo_s[:, j, 0:3]),
                    R=[R_hs[j]], W=[R_stg[sg]])
              kb.op(ACT, lambda b=b, sg=sg: se.activation(out=stg[:, sg, 3:515], in_=psb(b), func=AF.Copy),
                    R=[R_ps[b]], W=[R_stg[sg]])
              kb.op(DVE, lambda j=j, sg=sg: ve.tensor_copy(out=halo_s[:, j, 0:3], in_=stg[:, sg, 512:515]),
                    R=[R_stg[sg]], W=[R_hs[j]])
              return sl, rg, b, sg

          def xbc_b(j, ctx):
              sl, rg, b, sg = ctx
              b2 = 2 + b
              for k in range(4):
                  kb.op(PE, lambda k=k, b2=b2, sl=sl, sg=sg: te.matmul(
                      psb(b2), sl[:, 1024 + k * 128:1024 + (k + 1) * 128], stg[:, sg, k:k + 512],
                      start=(k == 0), stop=(k == 3)), R=[rg, R_stg[sg]], W=[R_ps[b2]])
              kb.op(ACT, lambda j=j, b2=b2: se.activation(out=xbcT[:, j, :], in_=psb(b2), func=AF.Silu,
                                                           bias=C("scb", j, j + 1)),
                    R=[R_ps[b2], R_cst], W=[R_xbc[j]])

          if t == 0:
              pipelined(range(12), xbc_a, xbc_b)
          chk("s1a")
          def qk_a(j):
              sl, rg = unit("qk", j)
              b = next_pa()
              sg = j % 2
              for kc in range(8):
                  kb.op(PE, lambda kc=kc, b=b, sl=sl: te.matmul(psb(b), sl[:, kc * 128:(kc + 1) * 128], xTb[:, kc, :],
                                                                start=(kc == 0), stop=(kc == 7)),
                        R=[rg] + R_xT, W=[R_ps[b]])
              kb.op(ACT, lambda b=b, sg=sg: se.activation(out=raw[:, sg, :], in_=psb(b), func=AF.Copy),
                    R=[R_ps[b]], W=[R_raw[sg]])
              return b, sg

          def qk_b(j, ctx):
              b, sg = ctx
              b2 = 2 + b
              kb.op(PE, lambda b2=b2, sg=sg: te.matmul(psb(b2), prot_bf[:, :], raw[:, sg, :], start=True, stop=True),
                    R=[R_c2, R_raw[sg]], W=[R_ps[b2]])
              kb.op(DVE, lambda b=b: ve.tensor_tensor(out=rt1[:, :], in0=psb(b), in1=cs_sb[:, 0:512], op=ALU.mult),
                    R=[R_ps[b], R_cs], W=[R_rt1])
              kb.op(DVE, lambda b2=b2: ve.tensor_tensor(out=rt2[:, :], in0=psb(b2), in1=cs_sb[:, 512:1024], op=ALU.mult),
                    R=[R_ps[b2], R_cs], W=[R_rt2])
              kb.op(DVE, lambda j=j: ve.tensor_tensor(out=qkT[:, j, :], in0=rt1[:, :], in1=rt2[:, :], op=ALU.add),
                    R=[R_rt1, R_rt2], W=[R_qk[j]])
              if j < 4:
                  kb.op(DVE, lambda j=j: ve.tensor_tensor(
                      out=qsT[:, j, :].rearrange("p (c n) -> p c n", c=4),
                      in0=qkT[:, j, :].rearrange("p (c n) -> p c n", c=4),
                      in1=C("qdecT", j * 128, (j + 1) * 128).unsqueeze(1).broadcast_to([128, 4, 128]), op=ALU.mult),
                      R=[R_qk[j], R_cst], W=[R_qs[j]])

          pipelined(range(8), qk_a, qk_b)
          chk("s1b")
          for j in range(8):
              sl, rg = unit("g", j)
              b = next_pa()
              for kc in range(8):
                  kb.op(PE, lambda kc=kc, b=b, sl=sl: te.matmul(psb(b), sl[:, kc * 128:(kc + 1) * 128], xTb[:, kc, :],
                                                                start=(kc == 0), stop=(kc == 7)),
                        R=[rg] + R_xT, W=[R_ps[b]])
              kb.op(ACT, lambda j=j, b=b: se.activation(out=gT[:, j, :], in_=psb(b), func=AF.Silu),
                    R=[R_ps[b]], W=[R_g[j]])
          chk("s1c")
          for kind, dst, regs, fn in (("z", zs, R_zs, AF.Silu), ("v", vt, R_v, AF.Copy)):
              for blk in range(2):
                  proj_tm_accum(kind, 2, lambda kc, c: xTb[:, kc, c * 128:(c + 1) * 128], lambda kc, c: [R_xT[c]], blk)
                  for c in range(4):
                      kb.op(ACT, lambda c=c, blk=blk, dst=dst, fn=fn: se.activation(
                          out=dst[:, c, blk * 512:(blk + 1) * 512], in_=psb(c), func=fn),
                          R=[R_ps[c]], W=[regs[c]])
          chk("s1d")
          sl, rg = unit("dt", 0)
          for c in range(4):
              for kc in range(8):
                  kb.op(PE, lambda kc=kc, c=c, sl=sl: te.matmul(
                      psb(4, c * 16, (c + 1) * 16), xTb[:, kc, c * 128:(c + 1) * 128], sl[:, kc * 16:(kc + 1) * 16],
                      start=(kc == 0), stop=(kc == 7)), R=[rg, R_xT[c]], W=[R_ps[4]])
          kb.op(DVE, lambda: ve.tensor_tensor(out=dt_tmp[:, :, :], in0=psb(4, 0, 64).rearrange("p (c h) -> p c h", c=4),
                                              in1=C("dtb").unsqueeze(1).broadcast_to([128, 4, 16]), op=ALU.add),
                R=[R_ps[4], R_cst], W=[R_dtt])
          kb.op(ACT, lambda: se.activation(out=dt_tmp[:, :, :], in_=dt_tmp[:, :, :], func=AF.Exp), R=[R_dtt], W=[R_dtt])
          kb.op(ACT, lambda: se.activation(out=dt_all[:, :, :], in_=dt_tmp[:, :, :], func=AF.Ln, bias=1.0),
                R=[R_dtt], W=[R_dt])
          kb.op(DVE, lambda: ve.tensor_tensor(out=a_all[:, :, :], in0=dt_all[:, :, :],
                                              in1=arow[:, :].unsqueeze(1).broadcast_to([128, 4, 16]), op=ALU.mult),
                R=[R_dt, R_c2], W=[R_a])

          chk("s1")
          def ssd_body(c):
              cs_ = slice(c * 128, (c + 1) * 128)
              for g in range(2):
                  kb.op(DVE, lambda g=g: ve.tensor_tensor(
                      out=rhs_a[:, g * 8:(g + 1) * 8, :], in0=C("tri").unsqueeze(1).broadcast_to([128, 8, 128]),
                      in1=a_all[:, c, g * 8:(g + 1) * 8].unsqueeze(2).broadcast_to([128, 8, 128]), op=ALU.mult),
                      R=[R_a, R_cst], W=[R_rhsa[g]])
                  for q in range(2):
                      b = g * 2 + q
                      kb.op(PE, lambda b=b, g=g, q=q: te.matmul(
                          psb(b), C("strict"),
                          rhs_a[:, g * 8 + q * 4:g * 8 + q * 4 + 4, :].rearrange("p h l -> p (h l)"),
                          start=True, stop=True), R=[R_cst, R_rhsa[g]], W=[R_ps[b]])
                  kb.op(ACT, lambda g=g: se.activation(out=segT[:, g * 8:(g + 1) * 8, :].rearrange("p h l -> p (h l)"),
                                                       in_=ps2(2 * g), func=AF.Exp),
                        R=[R_ps[2 * g], R_ps[2 * g + 1]], W=[R_seg[g]])
              for g in range(2):
                  kb.op(PE, lambda g=g: te.matmul(psb(4, g * 128, (g + 1) * 128), xbcT[:, 8 + g, cs_], xbcT[:, 10 + g, cs_],
                                                  start=True, stop=True),
                        R=[R_xbc[8 + g], R_xbc[10 + g]], W=[R_ps[4]])
              kb.op(DVE, lambda: ve.tensor_tensor(out=cbm[:, :, :], in0=psb(4, 0, 256).rearrange("p (g l) -> p g l", g=2),
                                                  in1=C("tri").unsqueeze(1).broadcast_to([128, 2, 128]), op=ALU.mult),
                    R=[R_ps[4], R_cst], W=[R_cbm])
              for g in range(2):
                  kb.op(DVE, lambda g=g: ve.tensor_tensor(
                      out=attT[:, g * 8:(g + 1) * 8, :], in0=segT[:, g * 8:(g + 1) * 8, :],
                      in1=cbm[:, g, :].unsqueeze(1).broadcast_to([128, 8, 128]), op=ALU.mult),
                      R=[R_seg[g], R_cbm], W=[R_att[g]])
              for j in range(8):
                  kb.op(PE, lambda j=j: te.transpose(out=psbf(5)[:, j * 128:(j + 1) * 128], in_=xbcT[:, j, cs_],
                                                     identity=ident_bf[:, :]),
                        R=[R_xbc[j], R_c2], W=[R_ps[5]])
              kb.op(DVE, lambda: ve.tensor_tensor(out=dtd[:, :], in0=dt_all[:, c, :], in1=segT[:, :, 127], op=ALU.mult),
                    R=[R_dt] + R_seg, W=[R_dtd])
              x3 = psbf(5).rearrange("p (h d) -> p h d", h=16)
              kb.op(DVE, lambda: ve.tensor_tensor(out=xdt[:, :].rearrange("p (h d) -> p h d", h=16), in0=x3,
                                                  in1=dt_all[:, c, :].unsqueeze(2).broadcast_to([128, 16, 64]),
                                                  op=ALU.mult), R=[R_ps[5], R_dt], W=[R_xdt])
              kb.op(DVE, lambda: ve.tensor_tensor(out=xdtd[:, :].rearrange("p (h d) -> p h d", h=16), in0=x3,
                                                  in1=dtd[:, :].unsqueeze(2).broadcast_to([128, 16, 64]),
                                                  op=ALU.mult), R=[R_ps[5], R_dtd], W=[R_xdtd])
              kb.op(DVE, lambda: ve.tensor_tensor(out=F1[:, :].rearrange("p (h d) -> p h d", h=16), in0=x3,
                                                  in1=C("drow").unsqueeze(2).broadcast_to([128, 16, 64]),
                                                  op=ALU.mult), R=[R_ps[5], R_cst], W=[R_F1])
              for g in range(2):
                  kb.op(PE, lambda g=g: te.transpose(out=psbf(4)[:, 512 + g * 128:512 + (g + 1) * 128],
                                                     in_=xbcT[:, 8 + g, cs_], identity=ident_bf[:, :]),
                        R=[R_xbc[8 + g], R_c2], W=[R_ps[4]])
              kb.op(ACT, lambda: se.activation(out=Btok[:, :, :].rearrange("p g n -> p (g n)"),
                                               in_=psbf(4)[:, 512:768], func=AF.Copy), R=[R_ps[4]], W=[R_Btok])
              kb.op(PE, lambda: te.matmul(psb(4, 384, 400), C("tri"), a_all[:, c, :], start=True, stop=True),
                    R=[R_cst, R_a], W=[R_ps[4]])
              kb.op(PE, lambda: te.matmul(psb(4, 400, 416), C("ones"), a_all[:, c, :], start=True, stop=True),
                    R=[R_cst, R_a], W=[R_ps[4]])
              kb.op(ACT, lambda: se.activation(out=dfcd[:, :], in_=psb(4, 384, 416), func=AF.Exp),
                    R=[R_ps[4]], W=[R_dfcd])
              for h in range(16):
                  kb.op(PE, lambda h=h: te.matmul(ps2(0)[:, h * 64:(h + 1) * 64], attT[:, h, :],
                                                  xdt[:, h * 64:(h + 1) * 64], start=True, stop=True),
                        R=[R_att[h // 8], R_xdt], W=[R_ps[h // 8]])
              for g in range(2):
                  kb.op(PE, lambda g=g: te.matmul(psb(2 + g), xbcT[:, 10 + g, cs_], hT_bf[:, g * 512:(g + 1) * 512],
                                                  start=True, stop=True),
                        R=[R_xbc[10 + g], R_hTb], W=[R_ps[2 + g]])
              kb.op(DVE, lambda: ve.tensor_tensor(out=F2[:, :].rearrange("p (h d) -> p h d", h=16),
                                                  in0=ps2(2).rearrange("p (h d) -> p h d", h=16),
                                                  in1=dfcd[:, 0:16].unsqueeze(2).broadcast_to([128, 16, 64]), op=ALU.mult),
                    R=[R_ps[2], R_ps[3], R_dfcd], W=[R_F2])
              kb.op(POOL, lambda: ge.tensor_tensor(out=F1[:, :], in0=F1[:, :], in1=F2[:, :], op=ALU.add),
                    R=[R_F1, R_F2], W=[R_F1])
              kb.op(DVE, lambda: ve.tensor_tensor(out=F1[:, :], in0=ps2(0), in1=F1[:, :], op=ALU.add),
                    R=[R_ps[0], R_ps[1], R_F1], W=[R_F1])
              kb.op(POOL, lambda: ge.tensor_tensor(out=F1[:, :], in0=F1[:, :], in1=zs[:, c, :], op=ALU.mult),
                    R=[R_F1, R_zs[c]], W=[R_F1])
              for g in range(2):
                  kb.op(ACT, lambda g=g: se.activation(out=asil[:, :], in_=F1[:, g * 512:(g + 1) * 512], func=AF.Square,
                                                       accum_out=sm[:, 8 + g:9 + g]), R=[R_F1], W=[R_asil, R_smS])
              kb.op(DVE, lambda: ve.tensor_scalar(out=sm[:, 8:10], in0=sm[:, 8:10], scalar1=1.0 / 512.0, scalar2=EPS,
                                                  op0=ALU.mult, op1=ALU.add), R=[R_smS], W=[R_smS])
              kb.op(ACT, lambda: se.activation(out=sm[:, 8:10], in_=sm[:, 8:10], func=AF.Ln), R=[R_smS], W=[R_smS])
              kb.op(ACT, lambda: se.activation(out=sm[:, 8:10], in_=sm[:, 8:10], func=AF.Exp, scale=-0.5),
                    R=[R_smS], W=[R_smS])
              for g in range(2):
                  kb.op(DVE, lambda g=g: ve.tensor_scalar(out=H1[:, g * 512:(g + 1) * 512], in0=F1[:, g * 512:(g + 1) * 512],
                                                          scalar1=sm[:, 8 + g:9 + g], scalar2=None, op0=ALU.mult),
                        R=[R_F1, R_smS], W=[R_H1])
              for j in range(8):
                  kb.op(PE, lambda j=j: te.transpose(out=psbf(5)[:, j * 128:(j + 1) * 128], in_=H1[:, j * 128:(j + 1) * 128],
                                                     identity=ident_bf[:, :]), R=[R_H1, R_c2], W=[R_ps[5]])
              kb.op(DVE, lambda: ve.tensor_tensor(out=ymixT[:, 0:8, cs_], in0=psbf(5).rearrange("p (j t) -> p j t", j=8),
                                                  in1=C("normw").unsqueeze(2).broadcast_to([128, 8, 128]), op=ALU.mult),
                    R=[R_ps[5], R_cst], W=[R_ym[c]])
              for g in range(2):
                  kb.op(PE, lambda g=g: te.matmul(psb(2 + g), Btok[:, g, :], xdtd[:, g * 512:(g + 1) * 512],
                                                  start=True, stop=True), R=[R_Btok, R_xdtd], W=[R_ps[2 + g]])
              kb.op(POOL, lambda: ge.tensor_tensor(out=hT[:, :].rearrange("p (h d) -> p h d", h=16),
                                                  in0=hT[:, :].rearrange("p (h d) -> p h d", h=16),
                                                  in1=dfcd[:, 16:32].unsqueeze(2).broadcast_to([128, 16, 64]),
                                                  op=ALU.mult), R=[R_hT, R_dfcd], W=[R_hT])
              kb.op(DVE, lambda: ve.tensor_tensor(out=hT[:, :], in0=ps2(2), in1=hT[:, :], op=ALU.add),
                    R=[R_hT, R_ps[2], R_ps[3]], W=[R_hT])
              kb.op(ACT, lambda: se.activation(out=hT_bf[:, :], in_=hT[:, :], func=AF.Copy), R=[R_hT], W=[R_hTb])

          def ret_body(c):
              cs_ = slice(c * 128, (c + 1) * 128)
              for j in range(4):
                  kb.op(PE, lambda j=j: te.transpose(out=psbf(6)[:, j * 128:(j + 1) * 128], in_=qkT[:, 4 + j, cs_],
                                                     identity=ident_bf[:, :]), R=[R_qk[4 + j], R_c2], W=[R_ps[6]])
              kb.op(DVE, lambda: ve.tensor_tensor(out=kd[:, :].rearrange("p (h d) -> p h d", h=8),
                                                  in0=psbf(6)[:, 0:512].rearrange("p (h d) -> p h d", h=8),
                                                  in1=C("kdec").unsqueeze(2).broadcast_to([128, 8, 64]), op=ALU.mult),
                    R=[R_ps[6], R_cst], W=[R_kd])
              for h in range(8):
                  p0 = (h % 2) * 64
                  kb.op(PE, lambda h=h, p0=p0: te.matmul(ps[p0:p0 + 64, 7, (h // 2) * 128:(h // 2 + 1) * 128],
                                                         kd[:, h * 64:(h + 1) * 64], vt[:, c, h * 128:(h + 1) * 128],
                                                         start=True, stop=True), R=[R_kd, R_v[c]], W=[R_ps[7]])
              kb.op(POOL, lambda: ge.tensor_tensor(out=rS[:, :].rearrange("p (j e) -> p j e", j=4),
                                                  in0=rS[:, :].rearrange("p (j e) -> p j e", j=4),
                                                  in1=C("cdr").unsqueeze(2).broadcast_to([128, 4, 128]), op=ALU.mult),
                    R=[R_rS, R_cst], W=[R_rS])
              kb.op(DVE, lambda: ve.tensor_tensor(out=rS[:, :], in0=psb(7), in1=rS[:, :], op=ALU.add),
                    R=[R_rS, R_ps[7]], W=[R_rS])
              for h in range(8):
                  p0 = (h % 2) * 64
                  kb.op(PE, lambda h=h, p0=p0: te.matmul(psb(6 + h % 2, (h // 2) * 128, (h // 2 + 1) * 128),
                                                         qkT[p0:p0 + 64, 4 + h // 2, cs_],
                                                         qkT[p0:p0 + 64, h // 2, cs_], start=True, stop=True),
                        R=[R_qk[4 + h // 2], R_qk[h // 2]], W=[R_ps[6 + h % 2]])
              kb.op(DVE, lambda: ve.tensor_tensor(
                  out=sT[:, :, :].rearrange("p (j par) n -> p par j n", par=2),
                  in0=ps[:, 6:8, :].rearrange("p par (j n) -> p par j n", j=4),
                  in1=C("decT").rearrange("p (j par n) -> p par j n", j=4, par=2), op=ALU.mult),
                    R=[R_ps[6], R_ps[7], R_cst], W=[R_sT])
              for h in range(8):
                  p0 = (h % 2) * 64
                  kb.op(PE, lambda h=h: te.matmul(ps2(6)[:, h * 128:(h + 1) * 128], sT[:, h, :],
                                                  vt[:, c, h * 128:(h + 1) * 128], start=True, stop=False),
                        R=[R_sT, R_v[c]], W=[R_ps[6 + h // 4]])
                  kb.op(PE, lambda h=h, p0=p0: te.matmul(ps2(6)[:, h * 128:(h + 1) * 128], qsT[p0:p0 + 64, h // 2, cs_],
                                                         rS_bf[p0:p0 + 64, (h // 2) * 128:(h // 2 + 1) * 128],
                                                         start=False, stop=True),
                        R=[R_qs[h // 2], R_rSb], W=[R_ps[6 + h // 4]])
              kb.op(ACT, lambda: se.activation(out=rS_bf[:, :], in_=rS[:, :], func=AF.Copy), R=[R_rS], W=[R_rSb])
              y3 = ps2(6).rearrange("p (h e) -> p h e", h=8)
              kb.op(DVE, lambda: ve.tensor_reduce(out=sm[:, 16:24], in_=y3, axis=AX.X, op=ALU.add),
                    R=[R_ps[6], R_ps[7]], W=[R_smR])
              kb.op(ACT, lambda: se.activation(out=pT[:, :, :].rearrange("p h e -> p (h e)"), in_=ps2(6), func=AF.Square),
                    R=[R_ps[6], R_ps[7]], W=[R_pT])
              kb.op(DVE, lambda: ve.tensor_reduce(out=sm[:, 24:32], in_=pT[:, :, :],
                                                  axis=AX.X, op=ALU.add), R=[R_pT], W=[R_smR])
              kb.op(DVE, lambda: ve.tensor_scalar(out=sm[:, 16:24], in0=sm[:, 16:24], scalar1=1.0 / 128.0, scalar2=None,
                                                  op0=ALU.mult), R=[R_smR], W=[R_smR])
              kb.op(DVE, lambda: ve.tensor_tensor(out=sm[:, 32:40], in0=sm[:, 16:24], in1=sm[:, 16:24], op=ALU.mult),
                    R=[R_smR], W=[R_smR])
              kb.op(DVE, lambda: ve.scalar_tensor_tensor(out=sm[:, 24:32], in0=sm[:, 24:32], scalar=1.0 / 128.0,
                                                         in1=sm[:, 32:40], op0=ALU.mult, op1=ALU.subtract),
                    R=[R_smR], W=[R_smR])
              kb.op(DVE, lambda: ve.tensor_scalar(out=sm[:, 24:32], in0=sm[:, 24:32], scalar1=EPS, scalar2=None,
                                                  op0=ALU.add), R=[R_smR], W=[R_smR])
              kb.op(ACT, lambda: se.activation(out=sm[:, 24:32], in_=sm[:, 24:32], func=AF.Ln), R=[R_smR], W=[R_smR])
              kb.op(ACT, lambda: se.activation(out=sm[:, 24:32], in_=sm[:, 24:32], func=AF.Exp, scale=-0.5),
                    R=[R_smR], W=[R_smR])
              kb.op(DVE, lambda: ve.scalar_tensor_tensor(out=sm[:, 32:40], in0=sm[:, 16:24], scalar=-1.0,
                                                         in1=sm[:, 24:32], op0=ALU.mult, op1=ALU.mult),
                    R=[R_smR], W=[R_smR])
              for h in range(8):
                  kb.op(ACT, lambda h=h: se.activation(out=H2[:, h * 128:(h + 1) * 128], in_=ps2(6)[:, h * 128:(h + 1) * 128],
                                                       func=AF.Identity, bias=sm[:, 32 + h:33 + h],
                                                       scale=sm[:, 24 + h:25 + h]),
                        R=[R_ps[6 + h // 4], R_smR], W=[R_H2])
              for h in range(8):
                  kb.op(PE, lambda h=h: te.transpose(out=psbf(6)[:, h * 128:(h + 1) * 128], in_=H2[:, h * 128:(h + 1) * 128],
                                                     identity=ident_bf[:, :]), R=[R_H2, R_c2], W=[R_ps[6]])
              for j in range(8):
                  kb.op(ACT, lambda j=j: se.activation(out=pT[:, j, :], in_=psbf(6)[:, j * 128:(j + 1) * 128],
                                                       func=AF.Identity, bias=C("gnb", j, j + 1),
                                                       scale=C("gnw", j, j + 1)),
                        R=[R_ps[6], R_cst], W=[R_pT])
              kb.op(DVE, lambda: ve.tensor_tensor(out=ymixT[:, 8:16, cs_], in0=pT[:, :, :], in1=gT[:, :, cs_], op=ALU.mult),
                    R=[R_pT] + R_g, W=[R_ym[c]])

          la = record(lambda: [ssd_body(c) for c in range(4)])
          lb = record(lambda: [ret_body(c) for c in range(4)])
          for th in merge_sched([la, lb]):
              th[0]()

          if dbg and t == 0:
              kb.dma(SP, "dbg_ymix", dbg_d["ymix"], ymixT[:, :, :].rearrange("p j t -> p (j t)"), R=R_ym)

          chk("s2")
          for half in range(2):
              proj_tm_accum("wout", 4, lambda kc, c: ymixT[:, kc, c * 128:(c + 1) * 128], lambda kc, c: [R_ym[c]], half)
              residual_from_psum(half)
          layer_norm_all("ln1g", "ln1b", True)
          if dbg and t == 0:
              kb.dma(SP, "dbg_x1", dbg_d["x1"], XB[:, :, :].rearrange("p c n -> p (c n)"), R=R_XB)

          chk("s3")
          kb.op(DVE, lambda: ve.memset(fence[:, 0:1], 0.0), R=view_a, W=view_b + [R_fence])

          for j in range(8):
              sl, rg = unit("xq", j)
              b = next_pa()
              for kc in range(8):
                  kb.op(PE, lambda kc=kc, b=b, sl=sl: te.matmul(psb(b), sl[:, kc * 128:(kc + 1) * 128], xTb[:, kc, :],
                                                                start=(kc == 0), stop=(kc == 7)),
                        R=[rg] + R_xT, W=[R_ps[b]])
              kb.op(ACT, lambda j=j, b=b: se.activation(out=qxT[:, j, :], in_=psb(b), func=AF.Copy),
                    R=[R_ps[b]], W=[R_qx[j]])
          def xattn_body(c, S):
              cs_ = slice(c * 128, (c + 1) * 128)
              b0, bt, Fp, R_Fp, Hp, R_Hp, Tp, R_Tp, so, R_so = S
              sc = ps[:, b0:b0 + 2, :].rearrange("p a b -> p (a b)")
              for h in range(4):
                  for dc in range(2):
                      kb.op(PE, lambda h=h, dc=dc: te.matmul(sc[:, h * 256:(h + 1) * 256], qxT[:, h * 2 + dc, cs_],
                                                             kmT[:, h * 2 + dc, :], start=(dc == 0), stop=(dc == 1)),
                            R=[R_qx[h * 2 + dc], R_km], W=[R_ps[b0 + h // 2]])
              kb.op(DVE, lambda: ve.tensor_reduce(out=sm[:, so:so + 4], in_=sc.rearrange("p (h m) -> p h m", h=4),
                                                  axis=AX.X, op=ALU.max), R=[R_ps[b0], R_ps[b0 + 1]], W=[R_so])
              kb.op(DVE, lambda: ve.tensor_scalar(out=sm[:, so:so + 4], in0=sm[:, so:so + 4], scalar1=-1.0 / 16.0,
                                                  scalar2=None, op0=ALU.mult), R=[R_so], W=[R_so])
              for h in range(4):
                  kb.op(ACT, lambda h=h: se.activation(out=Fp[:, h * 256:(h + 1) * 256], in_=sc[:, h * 256:(h + 1) * 256],
                                                       func=AF.Exp, bias=sm[:, so + h:so + h + 1], scale=1.0 / 16.0,
                                                       accum_out=sm[:, so + 4 + h:so + 5 + h]),
                        R=[R_ps[b0 + h // 2], R_so], W=[R_Fp, R_so])
              kb.op(DVE, lambda: ve.reciprocal(out=sm[:, so + 4:so + 8], in_=sm[:, so + 4:so + 8]), R=[R_so], W=[R_so])
              for h in range(4):
                  kb.op(DVE, lambda h=h: ve.tensor_scalar(out=Hp[:, h * 256:(h + 1) * 256], in0=Fp[:, h * 256:(h + 1) * 256],
                                                          scalar1=sm[:, so + 4 + h:so + 5 + h], scalar2=None, op0=ALU.mult),
                        R=[R_Fp, R_so], W=[R_Hp])
              for i in range(8):
                  kb.op(PE, lambda i=i: te.transpose(out=psbf(bt)[:, i * 128:(i + 1) * 128], in_=Hp[:, i * 128:(i + 1) * 128],
                                                     identity=ident_bf[:, :]), R=[R_Hp, R_c2], W=[R_ps[bt]])
              kb.op(ACT, lambda: se.activation(out=Tp[:, :, :].rearrange("p i l -> p (i l)"), in_=psbf(bt), func=AF.Copy),
                    R=[R_ps[bt]], W=[R_Tp])
              for h in range(4):
                  for dch in range(2):
                      i = h * 2 + dch
                      for mc in range(2):
                          kb.op(PE, lambda h=h, dch=dch, mc=mc, i=i: te.matmul(
                              sc[:, i * 128:(i + 1) * 128], vm[:, mc, h * 256 + dch * 128:h * 256 + (dch + 1) * 128],
                              Tp[:, h * 2 + mc, :], start=(mc == 0), stop=(mc == 1)),
                              R=[R_vm, R_Tp], W=[R_ps[b0 + i // 4]])
              kb.op(ACT, lambda: se.activation(out=oT[:, :, cs_], in_=sc.rearrange("p (i l) -> p i l", i=8),
                                               func=AF.Copy), R=[R_ps[b0], R_ps[b0 + 1]], W=[R_oT[c]])

          S_A = (0, 2, F2, R_F2, H2, R_H2, pT, R_pT, 40, R_sm)
          S_B = (3, 5, F1, R_F1, H1, R_H1, sT, R_sT, 56, R_smS)
          la = record(lambda: [xattn_body(c, S_A) for c in (0, 2)])
          lb = record(lambda: [xattn_body(c, S_B) for c in (1, 3)])
          for th in merge_sched([la, lb]):
              th[0]()
          for half in range(2):
              proj_tm_accum("xo", 2, lambda kc, c: oT[:, kc, c * 128:(c + 1) * 128], lambda kc, c: [R_oT[c]], half)
              residual_from_psum(half)
          layer_norm_all("ln2g", "ln2b", True)
          if dbg and t == 0:
              kb.dma(SP, "dbg_x2", dbg_d["x2"], XB[:, :, :].rearrange("p c n -> p (c n)"), R=R_XB)

          chk("s4")
          def ffn_a(it):
              j, part = it
              jj = part * 22 + j
              sl, rg = unit(("fa", "fu")[part], j, hold=1)
              b = next_pa()
              sg = part
              for kc in range(8):
                  kb.op(PE, lambda kc=kc, b=b, sl=sl: te.matmul(psb(b), sl[:, kc * 128:(kc + 1) * 128], xTb[:, kc, :],
                                                                start=(kc == 0), stop=(kc == 7)),
                        R=[rg] + R_xT, W=[R_ps[b]])
              kb.op(DVE, lambda jj=jj, sg=sg: ve.tensor_copy(out=stg[:, sg, 0:2], in_=halo_f[:, jj, 0:2]),
                    R=[R_hf[jj]], W=[R_stg[sg]])
              kb.op(ACT, lambda b=b, sg=sg, jj=jj: se.activation(out=stg[:, sg, 2:514], in_=psb(b), func=AF.Identity,
                                                                  bias=C("bup", jj, jj + 1)),
                    R=[R_ps[b], R_cst], W=[R_stg[sg]])
              kb.op(DVE, lambda jj=jj, sg=sg: ve.tensor_copy(out=halo_f[:, jj, 0:2], in_=stg[:, sg, 512:514]),
                    R=[R_stg[sg]], W=[R_hf[jj]])
              return sl, rg, b, sg, jj

          def ffn_b(it, ctx):
              j, part = it
              sl, rg, b, sg, jj = ctx
              b2 = 2 + b
              for k in range(3):
                  kb.op(PE, lambda k=k, b2=b2, sl=sl, sg=sg: te.matmul(
                      psb(b2), sl[:, 1024 + k * 128:1024 + (k + 1) * 128], stg[:, sg, k:k + 512],
                      start=(k == 0), stop=(k == 2)), R=[rg, R_stg[sg]], W=[R_ps[b2]])
              if part == 0:
                  kb.op(ACT, lambda b2=b2, jj=jj: se.activation(out=asil[:, :], in_=psb(b2), func=AF.Silu,
                                                                bias=C("fcb", jj, jj + 1)),
                        R=[R_ps[b2], R_cst], W=[R_asil])
              else:
                  kb.op(DVE, lambda b2=b2, jj=jj, j=j: ve.scalar_tensor_tensor(
                      out=gT2[:, j, :], in0=psb(b2), scalar=C("fcb", jj, jj + 1), in1=asil[:, :],
                      op0=ALU.add, op1=ALU.mult), R=[R_ps[b2], R_cst, R_asil], W=[R_g2[j]])

          pipelined([(j, part) for j in range(22) for part in (0, 1)], ffn_a, ffn_b)
          for half in range(2):
              for q in range(6):
                  sl, rg = unit("down", (half, q))
                  nk = 4 if q < 5 else 2
                  for c in range(4):
                      for k4 in range(nk):
                          kc = q * 4 + k4
                          kb.op(PE, lambda c=c, kc=kc, k4=k4, sl=sl: te.matmul(
                              psb(4 + c), gT2[:, kc, c * 128:(c + 1) * 128], sl[:, k4 * 512:(k4 + 1) * 512],
                              start=(kc == 0), stop=(kc == 21)), R=[rg, R_g2[kc]], W=[R_ps[4 + c]])
              for c in range(4):
                  kb.op(DVE, lambda c=c: ve.scalar_tensor_tensor(
                      out=XB[:, c, half * 512:(half + 1) * 512], in0=XB[:, c, half * 512:(half + 1) * 512],
                      scalar=ALPHA, in1=psb(4 + c), op0=ALU.mult, op1=ALU.add),
                      R=[R_XB[c], R_ps[4 + c]], W=[R_XB[c]])
          ln3 = lambda: layer_norm_all("ln3g", "ln3b", False, after=lambda c: kb.dma(
              POOL, f"xb{c}", out_d[t0 + c * 128:t0 + (c + 1) * 128, :], XB[:, c, :], R=[R_XB[c]]))
          if t + 1 < NT:
              prologue(t + 1)
              la = record(ln3)
              lb = record(lambda: pipelined(range(12), xbc_a, xbc_b))
              for th in merge_sched([la, lb]):
                  th[0]()
          else:
              ln3()

    except _Stop:
        pass
    kb.wait_all_dma(SP, list(kb.dsem.keys()))
    kb.es.close()
    return nc


def _core_inputs(inp, b, shared):
    ws, wo, cst, cs = shared
    x = np.asarray(inp["x"], np.float32)[b]
    mem = np.asarray(inp["mem"], np.float32)[b]
    return {
        "xT": np.ascontiguousarray(x.T.reshape(8, 128, L).transpose(1, 0, 2)),
        "xn": np.ascontiguousarray(x),
        "memT": np.ascontiguousarray(mem.T.reshape(8, 128, MEM).transpose(1, 0, 2)),
        "ws": ws, "wo": wo, "cst": cst, "cs": cs,
    }


def kernel(**inputs):
    shared = _host_prep(inputs)
    nc = _build()
    in_maps = [_core_inputs(inputs, b, shared) for b in range(8)]
    res = run_bass_kernel_spmd(nc, in_maps, core_ids=list(range(8)))
    return np.stack([np.asarray(r["out"], np.float32) for r in res.results], axis=0)
```
